# Optimizing a Trainium2 kernel written in Bass

```python
import math
import jax, jax.numpy as jnp
from jax import lax
import numpy as np

D_MODEL = 1024
BATCH = 8
SEQ = 2048
DEPTH = 1

RET_HEADS = 4
RET_HEAD_DIM = 128
RET_WIDTH = RET_HEADS * RET_HEAD_DIM
RET_CHUNK = 128
SWA_HEADS = 8
SWA_KV_HEADS = 2
SWA_GROUP = SWA_HEADS // SWA_KV_HEADS
SWA_HEAD_DIM = 64
SWA_WIDTH = SWA_HEADS * SWA_HEAD_DIM
SWA_KV_WIDTH = SWA_KV_HEADS * SWA_HEAD_DIM
WINDOW = 128
MIX_WIDTH = RET_WIDTH + SWA_WIDTH
IN_WIDTH = 4 * RET_WIDTH + SWA_WIDTH + 2 * SWA_KV_WIDTH
N_GROUPS = 4
EXPERTS_PER_GROUP = 8
N_EXPERTS = N_GROUPS * EXPERTS_PER_GROUP
TOP_K = 2
D_EXPERT = 512
MOE_BLOCK = 256
LN_EPS = 1e-5
GN_EPS = 1e-6
DEEPNORM_ALPHA = (2 * DEPTH) ** 0.25
DEEPNORM_BETA = (8 * DEPTH) ** -0.25

kernel_name = 'hymba_retention_swa_alibi_hmoe_deepnorm'


def layer_norm(x, g, b):
    xf = x.astype(jnp.float32)
    mu = xf.mean(-1, keepdims=True)
    var = jnp.square(xf - mu).mean(-1, keepdims=True)
    return ((xf - mu) * lax.rsqrt(var + LN_EPS) * g + b).astype(x.dtype)


def retention_chunkwise(q, k, v):
    bsz, nh, s_len, d = q.shape
    c = RET_CHUNK
    n = s_len // c
    log_g = jnp.log1p(-jnp.exp2(-5.0 - jnp.arange(nh, dtype=jnp.float32)))
    idx = jnp.arange(c, dtype=jnp.float32)
    diff = idx[:, None] - idx[None, :]
    decay_in = jnp.where(diff >= 0, jnp.exp(log_g[:, None, None] * jnp.maximum(diff, 0.0)), 0.0)
    q_decay = jnp.exp(log_g[:, None] * (idx + 1.0))
    k_decay = jnp.exp(log_g[:, None] * (c - 1.0 - idx))
    chunk_decay = jnp.exp(log_g * c)
    qc = q.reshape(bsz, nh, n, c, d)
    kc = k.reshape(bsz, nh, n, c, d)
    vc = v.reshape(bsz, nh, n, c, d)
    scores = jnp.einsum('bhncd,bhnmd->bhncm', qc, kc) * decay_in[None, :, None]
    inner = jnp.einsum('bhncm,bhnme->bhnce', scores, vc)
    kv = jnp.einsum('bhnmd,bhnme->nbhde', kc * k_decay[None, :, None, :, None], vc)

    def step(state, kv_n):
        return state * chunk_decay[None, :, None, None] + kv_n, state

    _, states = lax.scan(step, jnp.zeros_like(kv[0]), kv)
    cross = jnp.einsum('bhncd,nbhde->bhnce', qc * q_decay[None, :, None, :, None], states)
    return (inner + cross).reshape(bsz, nh, s_len, d)


def sliding_window_gqa(q, k, v, sinks):
    bsz, s_len = q.shape[:2]
    w = WINDOW
    n = s_len // w
    qb = q.reshape(bsz, n, w, SWA_KV_HEADS, SWA_GROUP, SWA_HEAD_DIM)
    pad = ((0, 0), (w, 0), (0, 0), (0, 0))
    kp = jnp.pad(k, pad).reshape(bsz, n + 1, w, SWA_KV_HEADS, SWA_HEAD_DIM)
    vp = jnp.pad(v, pad).reshape(bsz, n + 1, w, SWA_KV_HEADS, SWA_HEAD_DIM)
    kb = jnp.concatenate([kp[:, :-1], kp[:, 1:]], axis=2)
    vb = jnp.concatenate([vp[:, :-1], vp[:, 1:]], axis=2)
    s = jnp.einsum('bnqkgd,bnskd->bnkgqs', qb, kb,
                   preferred_element_type=jnp.float32) * (SWA_HEAD_DIM ** -0.5)
    qpos = jnp.arange(w)[:, None]
    kpos = jnp.arange(2 * w)[None, :] - w
    dist = qpos - kpos
    abs_k = jnp.arange(n)[:, None, None] * w + kpos[None]
    valid = (dist >= 0) & (dist < w) & (abs_k >= 0)
    slopes = jnp.exp2(-8.0 * (jnp.arange(SWA_HEADS, dtype=jnp.float32) + 1.0) / SWA_HEADS)
    slopes = slopes.reshape(SWA_KV_HEADS, SWA_GROUP)[:, :, None, None]
    s = s - slopes * dist.astype(jnp.float32)
    s = jnp.where(valid[None, :, None, None], s, -jnp.inf)
    sink = sinks.astype(jnp.float32).reshape(SWA_KV_HEADS, SWA_GROUP)[:, :, None, None]
    m = jnp.maximum(s.max(-1, keepdims=True), sink)
    p = jnp.exp(s - m)
    p = p / (p.sum(-1, keepdims=True) + jnp.exp(sink - m))
    o = jnp.einsum('bnkgqs,bnskd->bnqkgd', p, vb.astype(jnp.float32))
    return o.reshape(bsz, s_len, SWA_WIDTH).astype(q.dtype)


def hybrid_mixer(x, w_in, ret_gn_g, attn_sinks, w_out):
    bsz, s_len, _ = x.shape
    proj = jnp.einsum('bsd,de->bse', x, w_in)
    splits = [RET_WIDTH, 2 * RET_WIDTH, 3 * RET_WIDTH, 4 * RET_WIDTH,
              4 * RET_WIDTH + SWA_WIDTH, 4 * RET_WIDTH + SWA_WIDTH + SWA_KV_WIDTH]
    q_r, k_r, v_r, g_r, q_a, k_a, v_a = jnp.split(proj, splits, axis=-1)

    def heads(t):
        return t.reshape(bsz, s_len, RET_HEADS, RET_HEAD_DIM).transpose(0, 2, 1, 3).astype(jnp.float32)
    o_r = retention_chunkwise(heads(q_r), heads(k_r) * (RET_HEAD_DIM ** -0.5), heads(v_r))
    mu = o_r.mean(-1, keepdims=True)
    var = jnp.square(o_r - mu).mean(-1, keepdims=True)
    o_r = ((o_r - mu) * lax.rsqrt(var + GN_EPS)).transpose(0, 2, 1, 3).reshape(bsz, s_len, RET_WIDTH)
    o_r = (o_r * ret_gn_g).astype(x.dtype) * jax.nn.silu(g_r)

    o_a = sliding_window_gqa(
        q_a.reshape(bsz, s_len, SWA_KV_HEADS, SWA_GROUP, SWA_HEAD_DIM),
        k_a.reshape(bsz, s_len, SWA_KV_HEADS, SWA_HEAD_DIM),
        v_a.reshape(bsz, s_len, SWA_KV_HEADS, SWA_HEAD_DIM),
        attn_sinks)

    o = jnp.concatenate([o_r, o_a.astype(x.dtype)], axis=-1)
    return jnp.einsum('bse,ed->bsd', o, w_out)


def hierarchical_moe(h, w_group_router, b_group_router, w_expert_router, b_expert_router,
                     w_gate, w_up, w_down):
    t_len, d = h.shape
    gl = (h @ w_group_router + b_group_router).astype(jnp.float32)
    gp = jax.nn.softmax(gl, axis=-1)
    g_idx = jnp.argmax(gl, axis=-1)
    g_w = jnp.take_along_axis(gp, g_idx[:, None], axis=-1)
    el = jnp.einsum('td,gde->tge', h, w_expert_router) + b_expert_router
    el = jnp.take_along_axis(el, g_idx[:, None, None], axis=1)[:, 0].astype(jnp.float32)
    top_l, top_i = lax.top_k(el, TOP_K)
    e_w = jax.nn.softmax(top_l, axis=-1) * g_w
    e_id = g_idx[:, None] * EXPERTS_PER_GROUP + top_i

    flat_e = e_id.reshape(-1)
    flat_w = e_w.reshape(-1)
    flat_tok = jnp.repeat(jnp.arange(t_len), TOP_K)
    order = jnp.argsort(flat_e)
    e_s = flat_e[order]
    tok_s = flat_tok[order]
    w_s = flat_w[order]
    counts = jnp.bincount(flat_e, length=N_EXPERTS)
    padded = (counts + MOE_BLOCK - 1) // MOE_BLOCK * MOE_BLOCK
    start = jnp.cumsum(counts) - counts
    pend = jnp.cumsum(padded)
    pstart = pend - padded
    dest = pstart[e_s] + (jnp.arange(t_len * TOP_K) - start[e_s])
    n_blocks = -(-(t_len * TOP_K) // MOE_BLOCK) + N_EXPERTS
    n_rows = n_blocks * MOE_BLOCK
    rows = jnp.zeros((n_rows, d), h.dtype).at[dest].set(h[tok_s])
    block_expert = jnp.searchsorted(pend, jnp.arange(n_blocks) * MOE_BLOCK, side='right')
    block_expert = jnp.minimum(block_expert, N_EXPERTS - 1)

    def expert_block(args):
        xb, e = args
        return (jax.nn.silu(xb @ w_gate[e]) * (xb @ w_up[e])) @ w_down[e]

    y_rows = lax.map(expert_block, (rows.reshape(n_blocks, MOE_BLOCK, d), block_expert))
    y_rows = y_rows.reshape(n_rows, d)
    return jnp.zeros((t_len, d), h.dtype).at[tok_s].add(y_rows[dest] * w_s[:, None].astype(h.dtype))


def setup_inputs(seed: int = 0) -> dict:
    key = jax.random.key(seed)
    ks = jax.random.split(key, 16)
    f32 = jnp.float32

    def normal(k, shape, scale):
        return jax.random.normal(k, shape, f32) * scale

    col_scale = jnp.concatenate([
        jnp.ones((2 * RET_WIDTH,), f32),
        jnp.full((RET_WIDTH,), DEEPNORM_BETA, f32),
        jnp.ones((RET_WIDTH + SWA_WIDTH + SWA_KV_WIDTH,), f32),
        jnp.full((SWA_KV_WIDTH,), DEEPNORM_BETA, f32)])
    x = normal(ks[0], (BATCH, SEQ, D_MODEL), 1.0)
    w_in = normal(ks[1], (DEPTH, D_MODEL, IN_WIDTH), D_MODEL ** -0.5) * col_scale
    ret_gn_g = 1.0 + normal(ks[2], (DEPTH, RET_WIDTH), 0.02)
    attn_sinks = normal(ks[3], (DEPTH, SWA_HEADS), 0.5)
    w_out = normal(ks[4], (DEPTH, MIX_WIDTH, D_MODEL), MIX_WIDTH ** -0.5 * DEEPNORM_BETA)
    ln1_g = 1.0 + normal(ks[5], (DEPTH, D_MODEL), 0.02)
    ln1_b = normal(ks[6], (DEPTH, D_MODEL), 0.02)
    w_group_router = normal(ks[7], (DEPTH, D_MODEL, N_GROUPS), D_MODEL ** -0.5)
    b_group_router = normal(ks[8], (DEPTH, N_GROUPS), 0.01)
    w_expert_router = normal(ks[9], (DEPTH, N_GROUPS, D_MODEL, EXPERTS_PER_GROUP), D_MODEL ** -0.5)
    b_expert_router = normal(ks[10], (DEPTH, N_GROUPS, EXPERTS_PER_GROUP), 0.01)
    w_gate = normal(ks[11], (DEPTH, N_EXPERTS, D_MODEL, D_EXPERT), D_MODEL ** -0.5 * DEEPNORM_BETA)
    w_up = normal(ks[12], (DEPTH, N_EXPERTS, D_MODEL, D_EXPERT), D_MODEL ** -0.5 * DEEPNORM_BETA)
    w_down = normal(ks[13], (DEPTH, N_EXPERTS, D_EXPERT, D_MODEL), D_EXPERT ** -0.5 * DEEPNORM_BETA)
    ln2_g = 1.0 + normal(ks[14], (DEPTH, D_MODEL), 0.02)
    ln2_b = normal(ks[15], (DEPTH, D_MODEL), 0.02)
    return {'x': x, 'w_in': w_in, 'ret_gn_g': ret_gn_g, 'attn_sinks': attn_sinks,
            'w_out': w_out, 'ln1_g': ln1_g, 'ln1_b': ln1_b,
            'w_group_router': w_group_router, 'b_group_router': b_group_router,
            'w_expert_router': w_expert_router, 'b_expert_router': b_expert_router,
            'w_gate': w_gate, 'w_up': w_up, 'w_down': w_down,
            'ln2_g': ln2_g, 'ln2_b': ln2_b}


def reference(x, w_in, ret_gn_g, attn_sinks, w_out, ln1_g, ln1_b,
              w_group_router, b_group_router, w_expert_router, b_expert_router,
              w_gate, w_up, w_down, ln2_g, ln2_b):
    bsz, s_len, d = x.shape
    for i in range(DEPTH):
        mix = hybrid_mixer(x, w_in[i], ret_gn_g[i], attn_sinks[i], w_out[i])
        x = layer_norm(DEEPNORM_ALPHA * x + mix, ln1_g[i], ln1_b[i])
        ffn = hierarchical_moe(x.reshape(bsz * s_len, d), w_group_router[i], b_group_router[i],
                               w_expert_router[i], b_expert_router[i],
                               w_gate[i], w_up[i], w_down[i]).reshape(bsz, s_len, d)
        x = layer_norm(DEEPNORM_ALPHA * x + ffn, ln2_g[i], ln2_b[i])
    return x
```

```python
import os
import numpy as np
import ml_dtypes
import concourse.bass as bass
import concourse.mybir as mybir
from concourse.bass_utils import run_bass_kernel_spmd

F32 = mybir.dt.float32
BF16 = mybir.dt.bfloat16
I32 = mybir.dt.int32
ALU = mybir.AluOpType
AF = mybir.ActivationFunctionType
AX = mybir.AxisListType

S = 2048
D = 1024
NT = 16
INW = 2816
NE = 32
DE = 512
CAP = 256
NSLOT = NE * CAP
ALPHA = 2.0 ** 0.25
LN_EPS = 1e-5
GN_EPS = 1e-6
BIGIDX = 1.0e6
MERGE_B = False
SPW = 12

ENGINES = ['pe', 'act', 'dve', 'pool', 'sp']


class Op:
    pass


class Prog:
    def __init__(self):
        self.ops = []
        self.eng_ops = {e: [] for e in ENGINES}
        self.last_w = {}
        self.readers = {}
        self.phase = 0
        self.dma_cnt = {}
        self.dma_last = {}
        self.capture = None

    def op(self, eng, fn, R=(), W=(), dma=None):
        if self.capture is not None:
            self.capture.append((eng, fn, tuple(R), tuple(W), dma))
            return None
        o = Op()
        o.eng = eng
        o.fn = fn
        o.phase = self.phase
        o.is_dma = dma is not None
        o.key = dma
        o.signal = False
        o.sigval = 0
        deps = []
        for r in R:
            w = self.last_w.get(r)
            if w is not None:
                deps.append((w, 'raw'))
        for r in W:
            w = self.last_w.get(r)
            if w is not None:
                deps.append((w, 'waw'))
            for rd in self.readers.get(r, {}).values():
                deps.append((rd, 'war'))
        o.deps = deps
        if o.is_dma:
            c = self.dma_cnt.get(dma, 0) + 16
            self.dma_cnt[dma] = c
            o.keyval = c
            self.dma_last[dma] = o
        for r in R:
            self.readers.setdefault(r, {})[('d', dma) if o.is_dma else ('e', eng)] = o
        for r in W:
            self.last_w[r] = o
            self.readers[r] = {}
        self.ops.append(o)
        self.eng_ops[eng].append(o)
        return o

    def run_merged(self, lists):
        lists = [l for l in lists if l]
        wts = [[(it[1] if it[0] == '__sp__' else 1) for it in l] for l in lists]
        tot = [float(sum(w)) for w in wts]
        pos = [0] * len(lists)
        used = [0.0] * len(lists)
        remaining = sum(len(l) for l in lists)
        while remaining > 0:
            best, bestf = -1, 1e9
            for k, l in enumerate(lists):
                if pos[k] < len(l):
                    f = used[k] / tot[k]
                    if f < bestf:
                        best, bestf = k, f
            it = lists[best][pos[best]]
            used[best] += wts[best][pos[best]]
            pos[best] += 1
            remaining -= 1
            if it[0] != '__sp__':
                self.op(*it)

    def spacer(self, w):
        if self.capture is not None:
            self.capture.append(('__sp__', w))

    def barrier(self, engines=ENGINES):
        lasts = []
        for e in ENGINES:
            for o in reversed(self.eng_ops[e]):
                if not o.is_dma and o.fn is not None:
                    lasts.append((o, 'raw'))
                    break
        for k, o in self.dma_last.items():
            lasts.append((o, 'raw'))
        for e in engines:
            o = Op()
            o.eng = e
            o.fn = None
            o.phase = self.phase
            o.is_dma = False
            o.key = None
            o.signal = False
            o.sigval = 0
            o.deps = list(lasts)
            o.force = True
            self.ops.append(o)
            self.eng_ops[e].append(o)

    def analyze(self):
        for e, lst in self.eng_ops.items():
            for i, o in enumerate(lst):
                o.idx = i
        waited = {e: {} for e in ENGINES}
        for o in self.ops:
            need = {}
            for (p, kind) in o.deps:
                if p.is_dma:
                    k = ('dma', p.key)
                    v = p.keyval
                else:
                    if p.eng == o.eng and not o.is_dma:
                        if getattr(o, 'force', False):
                            continue
                        if not (kind == 'raw' and o.eng in ('act', 'dve', 'pool')):
                            continue
                    k = ('eng', p.eng)
                    v = p.idx
                if waited[o.eng].get(k, -1) >= v:
                    continue
                if k not in need or need[k][0] < v:
                    need[k] = (v, p)
            o.need = need
            for k, (v, p) in need.items():
                waited[o.eng][k] = v
                if not p.is_dma:
                    p.signal = True
        for e, lst in self.eng_ops.items():
            c = 0
            for o in lst:
                if (not o.is_dma) and o.signal:
                    c += 1
                    o.sigval = c
        for o in self.ops:
            o.waits = []
            for k, (v, p) in o.need.items():
                if p.is_dma:
                    o.waits.append(('dma_' + p.key, p.keyval))
                else:
                    o.waits.append(('eng_' + p.eng, p.sigval))


def _consts():
    c = {}
    bf = ml_dtypes.bfloat16
    idx = np.arange(128, dtype=np.float64)
    c['ident'] = np.eye(128, dtype=np.float32).astype(bf)
    c['ones'] = np.ones((128, 128), np.float32).astype(bf)
    c['ustrict'] = (idx[:, None] < idx[None, :]).astype(np.float32).astype(bf)
    gam = 1.0 - 2.0 ** (-5.0 - np.arange(4, dtype=np.float64))
    m01 = (idx[:, None] <= idx[None, :]).astype(np.float32)
    c['mask01'] = np.tile(m01, (1, 4)).astype(bf)
    vs = np.zeros((128, 512), np.float64)
    rs = np.zeros((128, 512), np.float64)
    gg = np.zeros((128, 512), np.float64)
    for h in range(4):
        vs[:, h * 128:(h + 1) * 128] = (gam[h] ** (-(idx + 1.0)))[:, None]
        rs[:, h * 128:(h + 1) * 128] = (gam[h] ** (idx + 1.0))[:, None] * (128.0 ** -0.5)
        gg[:, h * 128:(h + 1) * 128] = gam[h] ** 128.0
    c['vs'] = vs.astype(np.float32)
    c['rs'] = rs.astype(np.float32)
    c['gdec'] = gg.astype(np.float32)
    slopes = 2.0 ** (-8.0 * (np.arange(8, dtype=np.float64) + 1.0) / 8.0)
    ecur = np.zeros((128, 2, 4, 128), np.float64)
    eprev = np.zeros((128, 2, 4, 128), np.float64)
    j = idx[:, None]
    i = idx[None, :]
    for kh in range(2):
        for g in range(4):
            sl = slopes[kh * 4 + g]
            ecur[:, kh, g, :] = np.where(j <= i, np.exp(-sl * (i - j)), 0.0)
            eprev[:, kh, g, :] = np.where(j > i, np.exp(-sl * (i + 128.0 - j)), 0.0)
    c['ecur'] = ecur.reshape(128, 1024).astype(np.float32).astype(bf)
    c['eprev'] = eprev.reshape(128, 1024).astype(np.float32).astype(bf)
    eb = np.tile((np.arange(NE, dtype=np.float32) * CAP)[None, :], (128, NT))
    c['ebase'] = eb.astype(np.float32)
    bb = np.arange(2 * NE)
    pp = np.arange(128)[:, None]
    c['posb'] = ((bb % 2)[None, :] * 128 + pp).astype(np.float32)
    c['slotb'] = ((bb // 2)[None, :] * CAP + (bb % 2)[None, :] * 128 + pp).astype(np.float32)
    return c


CONST_SHAPES = {
    'ident': ([128, 128], BF16), 'ones': ([128, 128], BF16), 'ustrict': ([128, 128], BF16),
    'mask01': ([128, 512], BF16), 'vs': ([128, 512], F32), 'rs': ([128, 512], F32),
    'gdec': ([128, 512], F32), 'ecur': ([128, 1024], BF16), 'eprev': ([128, 1024], BF16),
    'ebase': ([128, NT * NE], F32), 'posb': ([128, 2 * NE], F32), 'slotb': ([128, 2 * NE], F32),
}
PARAM_SHAPES = {
    'gng': ([128, 512], F32), 'sk': ([128, 4], F32), 'sk8': ([128, 8], F32), 'ln1g': ([128, D], F32), 'ln1b': ([128, D], F32),
    'ln2g': ([128, D], F32), 'ln2b': ([128, D], F32), 'br': ([128, 36], F32), 'wr': ([D, 36], F32),
}


def build(stage=2):
    nc = bass.Bass("TRN2", target_bir_lowering=False)
    Dm = {}

    def dram(name, shape, dt, kind):
        Dm[name] = nc.dram_tensor(name, shape, dt, kind=kind).ap()

    dram('x', [S, D], F32, "ExternalInput")
    dram('w_in', [D, INW], F32, "ExternalInput")
    dram('w_out', [D, D], F32, "ExternalInput")
    dram('w_gate', [NE, D, DE], F32, "ExternalInput")
    dram('w_up', [NE, D, DE], F32, "ExternalInput")
    dram('w_down', [NE, DE, D], F32, "ExternalInput")
    for k, (shp, dt) in CONST_SHAPES.items():
        dram(k, shp, dt, "ExternalInput")
    for k, (shp, dt) in PARAM_SHAPES.items():
        dram(k, shp, dt, "ExternalInput")
    dram('out', [S, D], F32, "ExternalOutput")
    if stage == 1:
        dram('h32', [S, D], F32, "ExternalOutput")
    else:
        dram('h32', [S, D], F32, "Internal")
    dram('rows', [NSLOT, D], BF16, "Internal")
    dram('yrows', [NSLOT, D], F32, "Internal")

    P = Prog()
    T = {}

    specs = {}

    def sb(name, shape, dt, scope):
        specs[name] = (shape, dt, scope)

    NSTG = 5
    NX = 4
    for i in range(NSTG):
        sb('stg%d' % i, [128, 2048], F32, 'b')
    for i in range(3):
        sb('stgA%d' % i, [128, 2048], F32, 'a')
    for i in range(2):
        sb('Wg%d' % i, [128, 4096], BF16, 'b')
        sb('Wu%d' % i, [128, 4096], BF16, 'b')
        sb('Wd%d' % i, [128, 4096], BF16, 'b')
    sb('L', [128, NT * 36], F32, 'c')
    sb('ident', [128, 128], BF16, 'c')
    sb('ones', [128, 128], BF16, 'c')

    psf_i = [0]
    psb_i = [0]

    def next_psf():
        psf_i[0] = (psf_i[0] + 1) % 2
        return 'bk%d' % (6 + psf_i[0])

    def next_psb():
        psb_i[0] = (psb_i[0] + 1) % 2
        return 'psb%d' % psb_i[0]

    def make_rot(names):
        st = [0]

        def f():
            st[0] = (st[0] + 1) % len(names)
            return names[st[0]]
        return f

    rotB1 = make_rot(['bk0', 'bk1'])
    rotC2 = make_rot(['bk2', 'bk3'])

    def load_const(name, dst=None, eng='sp'):
        dst = dst or name
        P.op(eng, lambda e, n=name, d=dst: e.dma_start(out=T[d][:, :], in_=Dm[n][:, :]),
             R=(), W=(dst,), dma='c_' + dst)

    P.phase = 0
    sb('Win', [128, 8 * INW], BF16, 'a')
    sb('Wout', [128, 8 * D], BF16, 'a')
    sb('Wr', [128, 8 * 36], BF16, 'a')
    sb('Wrf', [128, 8 * 36], F32, 'a')
    for i in range(2):
        sb('xin%d' % i, [128, D], F32, 'a')
        sb('xb%d' % i, [128, D], BF16, 'a')
        sb('xT%d' % i, [128, D], BF16, 'a')
        sb('stm%d' % i, [128, 512], BF16, 'a')
        sb('orb%d' % i, [128, 512], BF16, 'a')
        sb('orT%d' % i, [128, D], BF16, 'a')
        sb('hb%d' % i, [128, D], BF16, 'a')
        sb('hT%d' % i, [128, D], BF16, 'a')
        for kh in range(2):
            sb('Pc%d_%d' % (kh, i), [128, 512], BF16, 'a')
            sb('Pp%d_%d' % (kh, i), [128, 512], BF16, 'a')
    for i in range(3):
        sb('xr%d' % i, [128, D], F32, 'a')
        sb('fm%d' % i, [128, 12 * 128], BF16, 'a')
        sb('kTa%d' % i, [128, 128], BF16, 'a')
        sb('kr%d' % i, [128, 512], BF16, 'a')
        sb('vp%d' % i, [128, 512], BF16, 'a')
        sb('tg%d' % i, [128, 512], F32, 'a')
    for i in range(4):
        sb('va%d' % i, [128, 130], BF16, 'a')
    sb('sk8', [128, 8], F32, 'a')
    sb('EST', [128, 8], F32, 'a')
    sb('dnt', [128, 8], F32, 'a')
    sb('rdt', [128, 8], F32, 'a')
    for i in range(2):
        sb('oab%d' % i, [128, 512], BF16, 'a')
    sb('stbf', [128, 512], BF16, 'a')
    for n_ in ('st32', 'tmp32', 'os', 'yn'):
        sb(n_, [128, 512], F32, 'a')
    sb('gst', [128, 24], F32, 'a')
    sb('gmv', [128, 8], F32, 'a')
    sb('grs', [128, 4], F32, 'a')
    sb('gnb', [128, 4], F32, 'a')
    sb('lst', [128, 12], F32, 'a')
    sb('lmv', [128, 2], F32, 'a')
    sb('lrs', [128, 1], F32, 'a')
    sb('lnb', [128, 1], F32, 'a')
    sb('gsq', [128, 4], F32, 'a')
    sb('lsq', [128, 1], F32, 'a')
    sb('epsg', [128, 1], F32, 'c')
    sb('epsl', [128, 1], F32, 'c')
    sb('thb', [128, 512], F32, 'a')
    for k in ('mask01', 'ecur', 'eprev'):
        sb(k, CONST_SHAPES[k][0], BF16, 'a')
    for k in ('vs', 'rs', 'gdec'):
        sb(k, [128, 512], F32, 'a')
    sb('gng', [128, 512], F32, 'a')
    sb('sk', [128, 4], F32, 'a')
    sb('ske', [128, 4], F32, 'a')
    sb('ln1g', [128, D], F32, 'a')
    sb('ln1b', [128, D], F32, 'a')
    sb('br', [128, 36], F32, 'a')

    load_const('ident')
    P.op('sp', lambda e: e.dma_start(out=T['xin0'][:, :], in_=Dm['x'][0:128, :]), R=(), W=('xin0',), dma='xin0')

    P.op('pool', lambda e: e.memset(T['epsg'][:, :], 4.0 * GN_EPS), R=(), W=('epsg',))
    P.op('pool', lambda e: e.memset(T['epsl'][:, :], LN_EPS), R=(), W=('epsl',))

    cast_rr = [0]

    def cast_op(dst_fn, src_fn, R, W, eng=None):
        if eng is None:
            eng = ('act', 'dve')[cast_rr[0] % 2]
            cast_rr[0] += 1
        if eng == 'act':
            P.op('act', lambda e: e.copy(out=dst_fn(), in_=src_fn()), R=R, W=W)
        elif eng == 'dve':
            P.op('dve', lambda e: e.tensor_copy(out=dst_fn(), in_=src_fn()), R=R, W=W)
        else:
            P.op('pool', lambda e: e.tensor_copy(out=dst_fn(), in_=src_fn()), R=R, W=W)

    stg_i = [0]

    stgA_i = [0]

    def next_stgA():
        s = stgA_i[0] % 3
        stgA_i[0] += 1
        return s

    def next_stg():
        s = stg_i[0] % NSTG
        stg_i[0] += 1
        return s

    HW = INW // 2
    for hh in range(2):
        for kc in range(8):
            s = next_stgA()
            P.op('sp', lambda e, kc=kc, hh=hh, s=s: e.dma_start(
                out=T['stgA%d' % s][:, 0:HW], in_=Dm['w_in'][kc * 128:(kc + 1) * 128, hh * HW:(hh + 1) * HW]),
                R=(), W=('stgA%d' % s,), dma='stgA%d' % s)
            cast_op(lambda kc=kc, hh=hh: T['Win'][:, kc * INW + hh * HW: kc * INW + (hh + 1) * HW],
                    lambda s=s: T['stgA%d' % s][:, 0:HW],
                    R=('stgA%d' % s,), W=('Win%d_%d' % (kc, hh),))
    WIN_RES = tuple('Win%d_%d' % (kc, hh) for kc in range(8) for hh in range(2))

    def win_res(c0, c1):
        hs = set()
        if c0 < HW:
            hs.add(0)
        if c1 > HW:
            hs.add(1)
        return tuple('Win%d_%d' % (kc, hh) for kc in range(8) for hh in sorted(hs))
    for k in ('ones', 'mask01', 'vs', 'rs', 'gdec', 'ecur', 'eprev', 'gng', 'sk8', 'br', 'ln1g', 'ln1b'):
        load_const(k)
    wout_slots = []
    for q in range(4):
        s_ = next_stgA()
        wout_slots.append(s_)
        if q < 3:
            P.op('sp', lambda e, q=q, s_=s_: e.dma_start(
                out=T['stgA%d' % s_][:, :].rearrange("p (c n) -> p c n", c=2),
                in_=Dm['w_out'][q * 256:(q + 1) * 256, :].rearrange("(c p) n -> p c n", p=128)),
                R=(), W=('stgA%d' % s_,), dma='stgA%d' % s_)

    def wout_casts():
        for q in range(4):
            s_ = wout_slots[q]
            if q == 3:
                P.op('sp', lambda e, q=q, s_=s_: e.dma_start(
                    out=T['stgA%d' % s_][:, :].rearrange("p (c n) -> p c n", c=2),
                    in_=Dm['w_out'][q * 256:(q + 1) * 256, :].rearrange("(c p) n -> p c n", p=128)),
                    R=(), W=('stgA%d' % s_,), dma='stgA%d' % s_)
            cast_op(lambda q=q: T['Wout'][:, q * 2048:(q + 1) * 2048], lambda s_=s_: T['stgA%d' % s_][:, :],
                    R=('stgA%d' % s_,), W=('Wout%d' % q,))
    WOUT_RES = tuple('Wout%d' % q for q in range(4))
    P.op('sp', lambda e: e.dma_start(out=T['Wrf'][:, :].rearrange("p (c n) -> p c n", c=8),
                                     in_=Dm['wr'][:, :].rearrange("(c p) n -> p c n", p=128)),
         R=(), W=('Wrf',), dma='c_Wrf')
    P.op('dve', lambda e: e.tensor_copy(out=T['Wr'][:, :], in_=T['Wrf'][:, :]), R=('Wrf',), W=('Wr',))
    P.op('act', lambda e: e.activation(out=T['EST'][:, :], in_=T['sk8'][:, :], func=AF.Exp), R=('sk8',), W=('EST',))
    for i in range(4):
        P.op('pool', lambda e, i=i: e.memset(T['va%d' % i][:, :], 1.0), R=(), W=('va%d' % i,))

    sb('zt', [128, 2048], BF16, 'a')
    P.op('pool', lambda e: e.memset(T['zt'][:, :], 0.0), R=(), W=('zt',))
    def zero_dma(q):
        P.op('pool', lambda e, q=q: e.dma_start(
            out=Dm['rows'][q * 256:(q + 1) * 256, :].rearrange("(p r) f -> p (r f)", p=128),
            in_=T['zt'][:, :]), R=('zt',), W=('rowsz%d' % q,), dma='zero')
    ROWSZ = tuple('rowsz%d' % q for q in range(32))

    def PB(bank):
        return T[bank][:, :].bitcast(BF16)

    def xload(n):
        s = n % 2
        P.op('sp', lambda e: e.dma_start(out=T['xin%d' % s][:, :], in_=Dm['x'][n * 128:(n + 1) * 128, :]),
             R=(), W=('xin%d' % s,), dma='xin%d' % s)

    def xrload(n):
        s = n % 3
        P.op('sp', lambda e: e.dma_start(out=T['xr%d' % s][:, :], in_=Dm['x'][n * 128:(n + 1) * 128, :]),
             R=(), W=('xr%d' % s,), dma='xr%d' % s)

    def T0(n):
        if n + 1 < NT:
            xload(n + 1)
        xin = 'xin%d' % (n % 2)
        xb = 'xb%d' % (n % 2)
        P.op('act', lambda e: e.copy(out=T[xb][:, :], in_=T[xin][:, :]), R=(xin,), W=(xb,))
        if n == 1:
            wout_casts()

    def T1(n):
        xb = 'xb%d' % (n % 2)
        xT = 'xT%d' % (n % 2)
        bk = 'bk0'
        for kc in range(8):
            P.op('pe', lambda e, kc=kc: e.transpose(out=PB(bk)[:, kc * 128:(kc + 1) * 128],
                                                    in_=T[xb][:, kc * 128:(kc + 1) * 128],
                                                    identity=T['ident'][:, :]),
                 R=(xb, 'ident'), W=(bk,))
        P.op('dve', lambda e: e.tensor_copy(out=T[xT][:, :], in_=PB(bk)[:, :]), R=(bk,), W=(xT,))

    rot2 = make_rot(['bk1', 'bk2'])
    rot4 = make_rot(['bk4', 'bk5'])

    def T2(n):
        s2 = n % 2
        s3 = n % 3
        xT = 'xT%d' % s2
        fm = 'fm%d' % s3
        kTa = 'kTa%d' % s3
        va = 'va%d' % (n % 4)
        kr = 'kr%d' % s3
        vp = 'vp%d' % s3
        tg = 'tg%d' % s3

        def fm_group(cols0, nch, evac_eng, dst_fn, dst_res):
            pf = rot2()
            for c in range(nch):
                for kc in range(8):
                    P.op('pe', lambda e, c=c, kc=kc: e.matmul(
                        T[pf][:, c * 128:(c + 1) * 128],
                        lhsT=T['Win'][:, kc * INW + cols0 + c * 128: kc * INW + cols0 + (c + 1) * 128],
                        rhs=T[xT][:, kc * 128:(kc + 1) * 128], start=(kc == 0), stop=(kc == 7)),
                        R=win_res(cols0, cols0 + nch * 128) + (xT,), W=(pf,))
            if evac_eng == 'act':
                P.op('act', lambda e: e.copy(out=dst_fn(), in_=T[pf][:, 0:nch * 128]), R=(pf,), W=(dst_res,))
            else:
                P.op('dve', lambda e: e.tensor_copy(out=dst_fn(), in_=T[pf][:, 0:nch * 128]), R=(pf,), W=(dst_res,))

        def tm_group(cols0, ncols):
            pf = rot2()
            for kc in range(8):
                P.op('pe', lambda e, kc=kc: e.matmul(
                    T[pf][:, 0:ncols], lhsT=T[xT][:, kc * 128:(kc + 1) * 128],
                    rhs=T['Win'][:, kc * INW + cols0: kc * INW + cols0 + ncols], start=(kc == 0), stop=(kc == 7)),
                    R=win_res(cols0, cols0 + ncols) + (xT,), W=(pf,))
            return pf

        fm_group(0, 4, 'act', lambda: T[fm][:, 0:512], fm + '_q')
        fm_group(512, 4, 'dve', lambda: T[fm][:, 512:1024], fm + '_k')
        pf1 = tm_group(512, 512)
        P.op('act', lambda e: e.copy(out=T[kr][:, :], in_=T[pf1][:, :]), R=(pf1,), W=(kr,))
        fm_group(2048, 4, 'act', lambda: T[fm][:, 1024:1536], fm + '_qa')
        pf2 = tm_group(1024, 512)
        P.op('dve', lambda e: e.tensor_tensor(out=T[vp][:, :], in0=T[pf2][:, :], in1=T['vs'][:, :], op=ALU.mult),
             R=(pf2, 'vs'), W=(vp,))
        fm_group(2560, 1, 'dve', lambda: T[kTa][:, :], kTa)
        pf3 = tm_group(1536, 512)
        P.op('act', lambda e: e.activation(out=T['thb'][:, :], in_=T[pf3][:, :], func=AF.Tanh, scale=0.5), R=(pf3,), W=('thb',))
        pf4 = tm_group(2688, 128)
        P.op('dve', lambda e: e.scalar_tensor_tensor(out=T['thb'][:, :], in0=T['thb'][:, :], scalar=1.0, in1=T[pf3][:, :],
                                                     op0=ALU.add, op1=ALU.mult), R=(pf3, 'thb'), W=('thb',))
        P.op('dve', lambda e: e.tensor_copy(out=T[va][:, :].rearrange("p (k c) -> p k c", k=2)[:, :, 0:64],
                                            in_=T[pf4][:, 0:128].rearrange("p (k c) -> p k c", k=2)), R=(pf4,), W=(va,))
        P.op('pool', lambda e: e.tensor_tensor(out=T[tg][:, :], in0=T['thb'][:, :], in1=T['gng'][:, :], op=ALU.mult),
             R=('thb', 'gng'), W=(tg,))

    def T3(n):
        s2 = n % 2
        s3 = n % 3
        p3 = (n - 1) % 3
        fm = 'fm%d' % s3
        kTa = 'kTa%d' % s3
        stm = 'stm%d' % s2
        bk = 'bk3'
        for h in range(4):
            P.op('pe', lambda e, h=h: e.matmul(
                T[bk][:, h * 128:(h + 1) * 128], lhsT=T[fm][:, 512 + h * 128: 512 + (h + 1) * 128],
                rhs=T[fm][:, h * 128:(h + 1) * 128], start=True, stop=True),
                R=(fm + '_q', fm + '_k'), W=(bk,))
        P.op('dve', lambda e: e.tensor_tensor(out=T[stm][:, :], in0=T[bk][:, :], in1=T['mask01'][:, :], op=ALU.mult),
             R=(bk, 'mask01'), W=(stm,))
        for kh in range(2):
            for which in ('c', 'p'):
                if which == 'p' and n == 0:
                    continue
                kt = kTa if which == 'c' else 'kTa%d' % p3
                P.op('pe', lambda e, kh=kh, kt=kt: e.matmul(
                    T[bk][:, :], lhsT=T[kt][kh * 64:(kh + 1) * 64, :],
                    rhs=T[fm][kh * 64:(kh + 1) * 64, 1024:1536], start=True, stop=True),
                    R=(kt, fm + '_qa'), W=(bk,))
                pn = 'P%s%d_%d' % (which, kh, s2)
                et = 'ecur' if which == 'c' else 'eprev'
                P.op('act', lambda e, pn=pn: e.activation(out=T[pn][:, :], in_=T[bk][:, :], func=AF.Exp, scale=0.125),
                     R=(bk,), W=(pn,))
                P.op('pool', lambda e, pn=pn, et=et, kh=kh: e.tensor_tensor(
                    out=T[pn][:, :], in0=T[pn][:, :], in1=T[et][:, kh * 512:(kh + 1) * 512], op=ALU.mult),
                    R=(pn, et), W=(pn,))

    def T4(n):
        s2 = n % 2
        s3 = n % 3
        fm = 'fm%d' % s3
        va = 'va%d' % (n % 4)
        vap = 'va%d' % ((n - 1) % 4)
        kr = 'kr%d' % s3
        vp = 'vp%d' % s3
        tg = 'tg%d' % s3
        stm = 'stm%d' % s2
        orb = 'orb%d' % s2
        po = rot4()
        for h in range(4):
            P.op('pe', lambda e, h=h: e.matmul(
                T[po][:, h * 128:(h + 1) * 128], lhsT=T[stm][:, h * 128:(h + 1) * 128],
                rhs=T[vp][:, h * 128:(h + 1) * 128], start=True, stop=(n == 0)),
                R=(stm, vp), W=(po,))
            if n > 0:
                P.op('pe', lambda e, h=h: e.matmul(
                    T[po][:, h * 128:(h + 1) * 128], lhsT=T[fm][:, h * 128:(h + 1) * 128],
                    rhs=T['stbf'][:, h * 128:(h + 1) * 128], start=False, stop=True),
                    R=(fm + '_q', 'stbf'), W=(po,))
        pkv = rot4()
        for h in range(4):
            P.op('pe', lambda e, h=h: e.matmul(
                T[pkv][:, h * 128:(h + 1) * 128], lhsT=T[kr][:, h * 128:(h + 1) * 128],
                rhs=T[vp][:, h * 128:(h + 1) * 128], start=True, stop=True),
                R=(kr, vp), W=(pkv,))
        if n == 0:
            P.op('dve', lambda e: e.tensor_tensor(out=T['st32'][:, :], in0=T[pkv][:, :], in1=T['gdec'][:, :], op=ALU.mult),
                 R=(pkv, 'gdec'), W=('st32',))
        else:
            P.op('dve', lambda e: e.tensor_tensor(out=T['tmp32'][:, :], in0=T[pkv][:, :], in1=T['st32'][:, :], op=ALU.add),
                 R=(pkv, 'st32'), W=('tmp32',))
            P.op('pool', lambda e: e.tensor_tensor(out=T['st32'][:, :], in0=T['tmp32'][:, :], in1=T['gdec'][:, :], op=ALU.mult),
                 R=('tmp32', 'gdec'), W=('st32',))
        if n + 1 < NT:
            P.op('act', lambda e: e.copy(out=T['stbf'][:, :], in_=T['st32'][:, :]), R=('st32',), W=('stbf',))
        P.op('dve', lambda e: e.tensor_tensor(out=T['os'][:, :], in0=T[po][:, :], in1=T['rs'][:, :], op=ALU.mult),
             R=(po, 'rs'), W=('os',))
        oab = 'oab%d' % s2
        for kh in range(2):
            pb_ = rot4()
            seq = ([('p', vap)] if n > 0 else []) + [('c', va)]
            for g in range(4):
                for idx_, (which, vt) in enumerate(seq):
                    pn = 'P%s%d_%d' % (which, kh, s2)
                    P.op('pe', lambda e, pn=pn, vt=vt, pb_=pb_, g=g, kh=kh, first=(idx_ == 0), last=(idx_ == len(seq) - 1): e.matmul(
                        T[pb_][:, g * 65:(g + 1) * 65], lhsT=T[pn][:, g * 128:(g + 1) * 128],
                        rhs=T[vt][:, kh * 65:(kh + 1) * 65], start=first, stop=last),
                        R=(pn, vt), W=(pb_,))
            oav = lambda pb_=pb_: T[pb_][:, 0:260].rearrange("p (g c) -> p g c", g=4)
            P.op('dve', lambda e, oav=oav, kh=kh: e.tensor_tensor(
                out=T['dnt'][:, kh * 4:(kh + 1) * 4], in0=oav()[:, :, 64], in1=T['EST'][:, kh * 4:(kh + 1) * 4], op=ALU.add),
                R=(pb_, 'EST'), W=('dnt%d' % kh,))
            P.op('dve', lambda e, kh=kh: e.reciprocal(out=T['rdt'][:, kh * 4:(kh + 1) * 4], in_=T['dnt'][:, kh * 4:(kh + 1) * 4]),
                 R=('dnt%d' % kh,), W=('rdt%d' % kh,))
            P.op('dve', lambda e, oav=oav, kh=kh: e.tensor_tensor(
                out=T[oab][:, kh * 256:(kh + 1) * 256].rearrange("p (g c) -> p g c", g=4), in0=oav()[:, :, 0:64],
                in1=T['rdt'][:, kh * 4:(kh + 1) * 4].unsqueeze(2).to_broadcast([128, 4, 64]), op=ALU.mult),
                R=(pb_, 'rdt%d' % kh), W=(oab + '_%d' % kh,))
        for h in range(4):
            P.op('dve', lambda e, h=h: e.bn_stats(out=T['gst'][:, h * 6:(h + 1) * 6], in_=T['os'][:, h * 128:(h + 1) * 128]),
                 R=('os',), W=('gst%d' % h,))
        for h in range(4):
            P.op('dve', lambda e, h=h: e.bn_aggr(out=T['gmv'][:, h * 2:(h + 1) * 2], in_=T['gst'][:, h * 6:(h + 1) * 6]),
                 R=('gst%d' % h,), W=('gmv%d' % h,))
        gmv_v = lambda: T['gmv'][:, :].rearrange("p (h s) -> p h s", h=4)
        GMV = ('gmv0', 'gmv1', 'gmv2', 'gmv3')
        P.spacer(SPW)
        P.op('act', lambda e: e.activation(out=T['gsq'][:, :], in_=gmv_v()[:, :, 1], func=AF.Sqrt, scale=4.0, bias=T['epsg'][:, 0:1]),
             R=GMV + ('epsg',), W=('gsq',))
        P.spacer(SPW)
        P.op('dve', lambda e: e.reciprocal(out=T['grs'][:, :], in_=T['gsq'][:, :]), R=('gsq',), W=('grs',))
        P.op('dve', lambda e: e.scalar_tensor_tensor(out=T['gnb'][:, :], in0=gmv_v()[:, :, 0], scalar=-1.0, in1=T['grs'][:, :],
                                                     op0=ALU.mult, op1=ALU.mult), R=GMV + ('grs',), W=('gnb',))
        P.spacer(SPW)
        for h in range(4):
            P.op('act', lambda e, h=h: e.activation(out=T['yn'][:, h * 128:(h + 1) * 128], in_=T['os'][:, h * 128:(h + 1) * 128],
                                                    func=AF.Identity, scale=T['grs'][:, h:h + 1], bias=T['gnb'][:, h:h + 1]),
                 R=('os', 'grs', 'gnb'), W=('yn',))
        P.spacer(SPW)
        P.op('pool', lambda e: e.tensor_tensor(out=T[orb][:, :], in0=T['yn'][:, :], in1=T[tg][:, :], op=ALU.mult),
             R=('yn', tg), W=(orb,))

    def T5(n):
        orb = 'orb%d' % (n % 2)
        oab = 'oab%d' % (n % 2)
        orT = 'orT%d' % (n % 2)
        bk = 'bk0'
        for h in range(4):
            P.op('pe', lambda e, h=h: e.transpose(out=PB(bk)[:, h * 128:(h + 1) * 128], in_=T[orb][:, h * 128:(h + 1) * 128],
                                                  identity=T['ident'][:, :]), R=(orb, 'ident'), W=(bk,))
        for c in range(4):
            P.op('pe', lambda e, c=c: e.transpose(out=PB(bk)[:, (4 + c) * 128:(5 + c) * 128], in_=T[oab][:, c * 128:(c + 1) * 128],
                                                  identity=T['ident'][:, :]), R=(oab + '_0', oab + '_1', 'ident'), W=(bk,))
        P.op('act', lambda e: e.copy(out=T[orT][:, :], in_=PB(bk)[:, :]), R=(bk,), W=(orT,))
        xrload(n)

    def T6(n):
        xin = 'xr%d' % (n % 3)
        orT = 'orT%d' % (n % 2)
        hb = 'hb%d' % (n % 2)
        bks = ['bk6', 'bk7']
        for half in range(2):
            for kc in range(8):
                src = (lambda kc=kc: T[orT][:, kc * 128:(kc + 1) * 128])
                P.op('pe', lambda e, half=half, kc=kc, src=src: e.matmul(
                    T[bks[half]][:, :], lhsT=src(), rhs=T['Wout'][:, kc * D + half * 512: kc * D + (half + 1) * 512],
                    start=(kc == 0), stop=(kc == 7)),
                    R=WOUT_RES + (orT,), W=(bks[half],))
            P.op('dve', lambda e, half=half: e.scalar_tensor_tensor(
                out=T[xin][:, half * 512:(half + 1) * 512], in0=T[xin][:, half * 512:(half + 1) * 512], scalar=ALPHA,
                in1=T[bks[half]][:, :], op0=ALU.mult, op1=ALU.add), R=(bks[half], xin), W=(xin + 'h%d' % half,))
        for half in range(2):
            P.op('dve', lambda e, half=half: e.bn_stats(out=T['lst'][:, half * 6:(half + 1) * 6],
                                                        in_=T[xin][:, half * 512:(half + 1) * 512]),
                 R=(xin + 'h%d' % half,), W=('lst%d' % half,))
        P.op('dve', lambda e: e.bn_aggr(out=T['lmv'][:, :], in_=T['lst'][:, :]), R=('lst0', 'lst1'), W=('lmv',))
        P.op('dve', lambda e: e.scalar_tensor_tensor(out=T[xin][:, :], in0=T[xin][:, :], scalar=T['lmv'][:, 0:1],
                                                     in1=T['ln1g'][:, :], op0=ALU.subtract, op1=ALU.mult),
             R=(xin, xin + 'h0', xin + 'h1', 'lmv', 'ln1g'), W=(xin, xin + 'h0', xin + 'h1'))
        P.spacer(SPW)
        P.op('act', lambda e: e.activation(out=T['lsq'][:, :], in_=T['lmv'][:, 1:2], func=AF.Sqrt, bias=T['epsl'][:, 0:1]),
             R=('lmv', 'epsl'), W=('lsq',))
        P.spacer(SPW)
        P.op('dve', lambda e: e.reciprocal(out=T['lrs'][:, :], in_=T['lsq'][:, :]), R=('lsq',), W=('lrs',))
        P.op('dve', lambda e: e.scalar_tensor_tensor(out=T[xin][:, :], in0=T[xin][:, :], scalar=T['lrs'][:, 0:1],
                                                     in1=T['ln1b'][:, :], op0=ALU.mult, op1=ALU.add),
             R=(xin, 'lrs', 'ln1b'), W=(xin,))
        P.spacer(SPW)
        P.op('sp', lambda e: e.dma_start(out=Dm['h32'][n * 128:(n + 1) * 128, :], in_=T[xin][:, :]),
             R=(xin,), W=('h32_%d' % n,), dma='h32st')
        P.op('act', lambda e: e.copy(out=T[hb][:, :], in_=T[xin][:, :]), R=(xin,), W=(hb,))

    def T7(n):
        hb = 'hb%d' % (n % 2)
        hT = 'hT%d' % (n % 2)
        bk = 'bk0'
        for kc in range(8):
            P.op('pe', lambda e, kc=kc: e.transpose(out=PB(bk)[:, kc * 128:(kc + 1) * 128], in_=T[hb][:, kc * 128:(kc + 1) * 128],
                                                    identity=T['ident'][:, :]), R=(hb, 'ident'), W=(bk,))
        P.op('dve', lambda e: e.tensor_copy(out=T[hT][:, :], in_=PB(bk)[:, :]), R=(bk,), W=(hT,))

    def T8(n):
        hT = 'hT%d' % (n % 2)
        bk = 'bk0'
        for kc in range(8):
            P.op('pe', lambda e, kc=kc: e.matmul(T[bk][:, 0:36], lhsT=T[hT][:, kc * 128:(kc + 1) * 128],
                                                 rhs=T['Wr'][:, kc * 36:(kc + 1) * 36], start=(kc == 0), stop=(kc == 7)),
                 R=(hT, 'Wr'), W=(bk,))
        P.op('dve', lambda e: e.tensor_tensor(out=T['L'][:, n * 36:(n + 1) * 36], in0=T[bk][:, 0:36], in1=T['br'][:, :], op=ALU.add),
             R=(bk, 'br'), W=('L%d' % n,))

    def captured(fn, *a):
        P.capture = []
        fn(*a)
        l = P.capture
        P.capture = None
        return l

    GROUPS = [[(T0, 0)], [(T1, 1), (T5, 5), (T7, 7), (T8, 8)], [(T2, 2)], [(T3, 3)], [(T4, 4)], [(T6, 6)]]

    def run_group(grp, i):
        for (st, k) in grp:
            if 0 <= i - k < NT:
                st(i - k)

    for i in range(NT + 8):
        lists = [captured(run_group, grp, i) for grp in GROUPS]
        P.run_merged(lists)

    H32_RES = tuple('h32_%d' % n for n in range(NT))
    L_RES = tuple('L%d' % n for n in range(NT))

    if stage >= 2:
        P.barrier()
        P.phase = 1
        TT = NT
        sb('ustrict', [128, 128], BF16, 'b')
        sb('ebase', [128, NT * NE], F32, 'b')
        sb('ln2g', [128, D], F32, 'b')
        sb('ln2b', [128, D], F32, 'b')
        sb('posb', [128, 2 * NE], F32, 'b')
        sb('slotb', [128, 2 * NE], F32, 'b')
        sb('cnt', [128, NE], F32, 'b')
        sb('bovf', [128, 2 * NE], F32, 'b')
        sb('bidx', [128, 2 * NE], I32, 'b')
        for k in ('ustrict', 'ebase', 'ln2g', 'ln2b', 'posb', 'slotb'):
            load_const(k)
        for i in range(3):
            P.op('pool', lambda e, i=i: e.memset(T['RW%d' % i][:, :], 0.0), R=(), W=('RW%d' % i,))

        def mkreg(e):
            T['bcreg'] = e.alloc_register("bcreg")
            return e.reg_mov(T['bcreg'], NSLOT - 1)
        P.op('pool', mkreg)

        WBN = {'w_gate': 'Wg', 'w_up': 'Wu', 'w_down': 'Wd'}

        def pieces_at(st):
            out = []
            if st % 2 != 0:
                e1 = (st + 1) // 2
                if 0 <= e1 < NE:
                    out += [(e1, 'w_gate', 0, 'act'), (e1, 'w_up', 0, 'dve')]
                e2 = (st - 1) // 2
                if 0 <= e2 < NE:
                    out += [(e2, 'w_down', 0, 'dve')]
            else:
                e1 = st // 2
                if 0 <= e1 < NE:
                    out += [(e1, 'w_gate', 1, 'act'), (e1, 'w_up', 1, 'dve')]
                e2 = st // 2 - 1
                if 0 <= e2 < NE:
                    out += [(e2, 'w_down', 1, 'dve')]
            return out

        piece_slot = {}

        def piece_dma(pc):
            ex, nm, hh, _ = pc
            s = next_stg()
            piece_slot[pc] = s
            if nm == 'w_down':
                P.op('sp', lambda e: e.dma_start(
                    out=T['stg%d' % s][:, :].rearrange("p (c n) -> p c n", c=2),
                    in_=Dm[nm][ex, hh * 256:(hh + 1) * 256, :].rearrange("(c p) n -> p c n", p=128)),
                    R=(), W=('stg%d' % s,), dma='stg%d' % s)
            else:
                P.op('sp', lambda e: e.dma_start(
                    out=T['stg%d' % s][:, :].rearrange("p (c n) -> p c n", c=4),
                    in_=Dm[nm][ex, hh * 512:(hh + 1) * 512, :].rearrange("(c p) n -> p c n", p=128)),
                    R=(), W=('stg%d' % s,), dma='stg%d' % s)

        def piece_cast(pc):
            ex, nm, hh, eng = pc
            s = piece_slot[pc]
            wb = WBN[nm]
            sl = ex % 2
            cast_op(lambda: T['%s%d' % (wb, sl)][:, hh * 2048:(hh + 1) * 2048], lambda: T['stg%d' % s][:, :],
                    R=('stg%d' % s,), W=('%s%d_%d' % (wb, sl, hh),), eng=eng)


        def hload(t):
            s = t % 3
            P.op('sp', lambda e: e.dma_start(out=T['hl%d' % s][:, :], in_=Dm['h32'][t * 128:(t + 1) * 128, :]),
                 R=H32_RES, W=('hl%d' % s,), dma='hl%d' % s)

        hload(0)
        hload(1)

        EARLY = 2
        _pend = []
        for st_ in range(-1, EARLY + 1):
            for pc in pieces_at(st_):
                piece_dma(pc)
                _pend.append(pc)
                if len(_pend) > NSTG - 2:
                    piece_cast(_pend.pop(0))
        for pc in _pend:
            piece_cast(pc)
        for n_, w in (('gmax', TT), ('goh', TT * 4), ('gsh', TT * 4), ('gex', TT * 4), ('gsum', TT), ('gw', TT),
                      ('elm', TT * 32), ('els', TT * 8), ('m1', TT), ('oh1', TT * 8), ('els2', TT * 8), ('m2', TT),
                      ('oh2', TT * 8), ('dd', TT), ('ed', TT), ('ed1', TT), ('w1', TT), ('W1', TT), ('W2', TT),
                      ('A1', TT * 32), ('A2', TT * 32), ('Asum', TT * 32), ('CT', TT * 32), ('rank', TT * 32),
                      ('ovf', TT * 32), ('pos', TT * 32), ('pm1', TT * 32), ('d1f', TT), ('d2f', TT)):
            sb(n_, [128, w], F32, 'b')
        sb('Abf', [128, TT * 32], BF16, 'b')
        sb('d1i', [128, TT], I32, 'b')
        sb('d2i', [128, TT], I32, 'b')
        for i in range(3):
            sb('hbb%d' % i, [128, D], BF16, 'b')
        for i in range(2):
            sb('sgf%d' % i, [128, 512], F32, 'b')
        for i in range(3):
            sb('hl%d' % i, [128, D], F32, 'b')
            sb('XT%d' % i, [128, D], BF16, 'b')
            sb('Hh%d' % i, [128, 512], BF16, 'b')
            sb('HT%d' % i, [128, 512], BF16, 'b')
            sb('YS%d' % i, [128, D], F32, 'b')
            sb('Y1_%d' % i, [128, D], F32, 'b')
            sb('Y2_%d' % i, [128, D], F32, 'b')
            sb('RW%d' % i, [128, D], BF16, 'b')
        sb('lst2', [128, 12], F32, 'b')
        sb('lmv2', [128, 2], F32, 'b')
        sb('lrs2', [128, 1], F32, 'b')
        sb('lnb2', [128, 1], F32, 'b')
        sb('lsq2', [128, 1], F32, 'b')

        def v3(name, a, b):
            return T[name][:, :].rearrange("p (a b) -> p a b", a=a)

        def v4(name, a, b, c):
            return T[name][:, :].rearrange("p (a b c) -> p a b c", a=a, b=b)

        Lv = lambda: T['L'][:, :].rearrange("p (t c) -> p t c", t=TT)
        gl = lambda: Lv()[:, :, 0:4]
        el = lambda: Lv()[:, :, 4:36].rearrange("p t (g e) -> p t g e", g=4)

        def dve(fn, R, W):
            P.op('dve', fn, R=R, W=W)

        dve(lambda e: e.tensor_reduce(out=T['gmax'][:, :], in_=gl(), axis=AX.X, op=ALU.max), L_RES, ('gmax',))
        dve(lambda e: e.tensor_tensor(out=v3('goh', TT, 4), in0=gl(), in1=T['gmax'][:, :].unsqueeze(2).to_broadcast([128, TT, 4]),
                                      op=ALU.is_equal), L_RES + ('gmax',), ('goh',))
        dve(lambda e: e.tensor_tensor(out=v3('gsh', TT, 4), in0=gl(), in1=T['gmax'][:, :].unsqueeze(2).to_broadcast([128, TT, 4]),
                                      op=ALU.subtract), L_RES + ('gmax',), ('gsh',))
        P.op('act', lambda e: e.activation(out=T['gex'][:, :], in_=T['gsh'][:, :], func=AF.Exp), R=('gsh',), W=('gex',))
        dve(lambda e: e.tensor_reduce(out=T['gsum'][:, :], in_=v3('gex', TT, 4), axis=AX.X, op=ALU.add), ('gex',), ('gsum',))
        dve(lambda e: e.reciprocal(out=T['gw'][:, :], in_=T['gsum'][:, :]), ('gsum',), ('gw',))
        dve(lambda e: e.tensor_tensor(out=v4('elm', TT, 4, 8), in0=el(),
                                      in1=v3('goh', TT, 4).unsqueeze(3).to_broadcast([128, TT, 4, 8]), op=ALU.mult),
            L_RES + ('goh',), ('elm',))
        dve(lambda e: e.tensor_reduce(out=v3('els', TT, 8), in_=v4('elm', TT, 4, 8).rearrange("p t g e -> p t e g"),
                                      axis=AX.X, op=ALU.add), ('elm',), ('els',))
        dve(lambda e: e.tensor_reduce(out=T['m1'][:, :], in_=v3('els', TT, 8), axis=AX.X, op=ALU.max), ('els',), ('m1',))
        dve(lambda e: e.tensor_tensor(out=v3('oh1', TT, 8), in0=v3('els', TT, 8),
                                      in1=T['m1'][:, :].unsqueeze(2).to_broadcast([128, TT, 8]), op=ALU.is_equal),
            ('els', 'm1'), ('oh1',))
        dve(lambda e: e.scalar_tensor_tensor(out=T['els2'][:, :], in0=T['oh1'][:, :], scalar=-1.0e30, in1=T['els'][:, :],
                                             op0=ALU.mult, op1=ALU.add), ('oh1', 'els'), ('els2',))
        dve(lambda e: e.tensor_reduce(out=T['m2'][:, :], in_=v3('els2', TT, 8), axis=AX.X, op=ALU.max), ('els2',), ('m2',))
        dve(lambda e: e.tensor_tensor(out=v3('oh2', TT, 8), in0=v3('els2', TT, 8),
                                      in1=T['m2'][:, :].unsqueeze(2).to_broadcast([128, TT, 8]), op=ALU.is_equal),
            ('els2', 'm2'), ('oh2',))
        dve(lambda e: e.tensor_tensor(out=T['dd'][:, :], in0=T['m2'][:, :], in1=T['m1'][:, :], op=ALU.subtract), ('m1', 'm2'), ('dd',))
        P.op('act', lambda e: e.activation(out=T['ed'][:, :], in_=T['dd'][:, :], func=AF.Exp), R=('dd',), W=('ed',))
        dve(lambda e: e.tensor_scalar(out=T['ed1'][:, :], in0=T['ed'][:, :], scalar1=1.0, scalar2=None, op0=ALU.add), ('ed',), ('ed1',))
        dve(lambda e: e.reciprocal(out=T['w1'][:, :], in_=T['ed1'][:, :]), ('ed1',), ('w1',))
        dve(lambda e: e.tensor_tensor(out=T['W1'][:, :], in0=T['w1'][:, :], in1=T['gw'][:, :], op=ALU.mult), ('w1', 'gw'), ('W1',))
        dve(lambda e: e.tensor_tensor(out=T['W2'][:, :], in0=T['ed'][:, :], in1=T['W1'][:, :], op=ALU.mult), ('ed', 'W1'), ('W2',))
        for a_, oh in (('A1', 'oh1'), ('A2', 'oh2')):
            dve(lambda e, a_=a_, oh=oh: e.tensor_tensor(
                out=v4(a_, TT, 4, 8), in0=v3('goh', TT, 4).unsqueeze(3).to_broadcast([128, TT, 4, 8]),
                in1=v3(oh, TT, 8).unsqueeze(2).to_broadcast([128, TT, 4, 8]), op=ALU.mult), ('goh', oh), (a_,))
        dve(lambda e: e.tensor_tensor(out=T['Asum'][:, :], in0=T['A1'][:, :], in1=T['A2'][:, :], op=ALU.add), ('A1', 'A2'), ('Asum',))
        dve(lambda e: e.tensor_copy(out=T['Abf'][:, :], in_=T['Asum'][:, :]), ('Asum',), ('Abf',))
        pR = next_psf()
        pT = next_psf()
        P.op('pe', lambda e, pR=pR: e.matmul(T[pR][:, :], lhsT=T['ustrict'][:, :], rhs=T['Abf'][:, :], start=True, stop=True),
             R=('ustrict', 'Abf'), W=(pR,))
        P.op('pe', lambda e, pT=pT: e.matmul(T[pT][:, :], lhsT=T['ones'][:, :], rhs=T['Abf'][:, :], start=True, stop=True),
             R=('ones', 'Abf'), W=(pT,))
        dve(lambda e: e.memset(T['CT'][:, 0:32], 0.0), (), ('CT',))
        for t in range(1, TT):
            dve(lambda e, t=t, pT=pT: e.tensor_tensor(out=T['CT'][:, t * 32:(t + 1) * 32], in0=T['CT'][:, (t - 1) * 32:t * 32],
                                                      in1=T[pT][:, (t - 1) * 32:t * 32], op=ALU.add), (pT, 'CT'), ('CT',))
        dve(lambda e, pR=pR: e.tensor_tensor(out=T['rank'][:, :], in0=T[pR][:, :], in1=T['CT'][:, :], op=ALU.add), (pR, 'CT'), ('rank',))
        dve(lambda e: e.tensor_scalar(out=T['ovf'][:, :], in0=T['rank'][:, :], scalar1=float(CAP), scalar2=BIGIDX,
                                      op0=ALU.is_ge, op1=ALU.mult), ('rank',), ('ovf',))
        dve(lambda e: e.tensor_tensor(out=T['pos'][:, :], in0=T['rank'][:, :], in1=T['ebase'][:, :], op=ALU.add), ('rank', 'ebase'), ('pos',))
        dve(lambda e: e.tensor_tensor(out=T['pos'][:, :], in0=T['pos'][:, :], in1=T['ovf'][:, :], op=ALU.add), ('pos', 'ovf'), ('pos',))
        for a_, df, di in (('A1', 'd1f', 'd1i'), ('A2', 'd2f', 'd2i')):
            dve(lambda e, a_=a_: e.tensor_tensor(out=T['pm1'][:, :], in0=T[a_][:, :], in1=T['pos'][:, :], op=ALU.mult), (a_, 'pos'), ('pm1',))
            dve(lambda e, df=df: e.tensor_reduce(out=T[df][:, :], in_=v3('pm1', TT, 32), axis=AX.X, op=ALU.add), ('pm1',), (df,))
            dve(lambda e, df=df, di=di: e.tensor_copy(out=T[di][:, :], in_=T[df][:, :]), (df,), (di,))

        dve(lambda e, pT=pT: e.tensor_tensor(out=T['cnt'][:, :], in0=T['CT'][:, (TT - 1) * 32:TT * 32],
                                             in1=T[pT][:, (TT - 1) * 32:TT * 32], op=ALU.add), (pT, 'CT'), ('cnt',))
        dve(lambda e: e.tensor_tensor(out=T['bovf'][:, :].rearrange("p (e j) -> p e j", j=2),
                                      in0=T['posb'][:, :].rearrange("p (e j) -> p e j", j=2),
                                      in1=T['cnt'][:, :].unsqueeze(2).to_broadcast([128, NE, 2]), op=ALU.is_ge),
            ('posb', 'cnt'), ('bovf',))
        dve(lambda e: e.scalar_tensor_tensor(out=T['bovf'][:, :], in0=T['bovf'][:, :], scalar=BIGIDX, in1=T['slotb'][:, :],
                                             op0=ALU.mult, op1=ALU.add), ('bovf', 'slotb'), ('bovf',))
        dve(lambda e: e.tensor_copy(out=T['bidx'][:, :], in_=T['bovf'][:, :]), ('bovf',), ('bidx',))

        SC_RES = []

        for t in range(TT):
            s = t % 3
            s2 = t % 3
            if t + 2 < TT:
                hload(t + 2)
            P.op('act', lambda e, s=s, s2=s2: e.copy(out=T['hbb%d' % s2][:, :], in_=T['hl%d' % s][:, :]),
                 R=('hl%d' % s,), W=('hbb%d' % s2,))
            for k, di in ((1, 'd1i'), (2, 'd2i')):
                res = 'rows_s%d_%d' % (t, k)
                SC_RES.append(res)
                P.op('pool', lambda e, t=t, s2=s2, di=di: e.indirect_dma_start(
                    out=Dm['rows'][:, :], out_offset=bass.IndirectOffsetOnAxis(ap=T[di][:, t:t + 1], axis=0),
                    in_=T['hbb%d' % s2][:, :], in_offset=None, bounds_check=T['bcreg'], oob_is_err=False),
                    R=('hbb%d' % s2, di), W=(res,), dma='scat')
        SC_RES = tuple(SC_RES)

        def wres(wb, sl):
            return ('%s%d_0' % (wb, sl), '%s%d_1' % (wb, sl))

        NB = NE * 2
        Y_RES = ['yrows%d' % b for b in range(NB)]

        def stA(b):
            r3 = b % 3
            slot0 = (b // 2) * CAP + (b % 2) * 128
            rw = 'RW%d' % r3
            pb = 'bk4'
            for kc in range(8):
                P.op('pe', lambda e, kc=kc: e.transpose(out=PB(pb)[:, kc * 128:(kc + 1) * 128],
                                                        in_=T[rw][:, kc * 128:(kc + 1) * 128],
                                                        identity=T['ident'][:, :]), R=(rw, 'ident'), W=(pb,))
            xt = 'XT%d' % r3
            P.op('dve', lambda e: e.tensor_copy(out=T[xt][:, :], in_=PB(pb)[:, :]), R=(pb,), W=(xt,))

        def rowload(b):
            r3 = b % 3
            slot0 = (b // 2) * CAP + (b % 2) * 128
            rw = 'RW%d' % r3
            P.op('pool', lambda e: e.indirect_dma_start(
                out=T[rw][:, :], out_offset=None, in_=Dm['rows'][:, :],
                in_offset=bass.IndirectOffsetOnAxis(ap=T['bidx'][:, b:b + 1], axis=0),
                bounds_check=T['bcreg'], oob_is_err=False), R=SC_RES + ('bidx',), W=(rw,), dma=rw)

        def stB1(b):
            r3 = b % 3
            sl = (b // 2) % 2
            xt = 'XT%d' % r3
            pg = rotB1()
            pu = rotB1()
            for (pp, wb) in ((pg, 'Wg'), (pu, 'Wu')):
                for kc in range(8):
                    P.op('pe', lambda e, kc=kc, pp=pp, wb=wb: e.matmul(
                        T[pp][:, :], lhsT=T[xt][:, kc * 128:(kc + 1) * 128],
                        rhs=T['%s%d' % (wb, sl)][:, kc * 512:(kc + 1) * 512], start=(kc == 0), stop=(kc == 7)),
                        R=(xt,) + wres(wb, sl), W=(pp,))
            sgf = 'sgf%d' % (b % 2)
            P.op('act', lambda e: e.activation(out=T[sgf][:, :], in_=T[pg][:, :], func=AF.Silu), R=(pg,), W=(sgf,))
            hh_ = 'Hh%d' % r3
            P.op('dve', lambda e: e.tensor_tensor(out=T[hh_][:, :], in0=T[pu][:, :], in1=T[sgf][:, :], op=ALU.mult),
                 R=(pu, sgf), W=(hh_,))

        def stB2(b):
            r3 = b % 3
            hh_ = 'Hh%d' % r3
            pb = 'bk5'
            for c in range(4):
                P.op('pe', lambda e, c=c: e.transpose(out=PB(pb)[:, c * 128:(c + 1) * 128],
                                                      in_=T[hh_][:, c * 128:(c + 1) * 128],
                                                      identity=T['ident'][:, :]), R=(hh_, 'ident'), W=(pb,))
            ht = 'HT%d' % r3
            P.op('act', lambda e: e.copy(out=T[ht][:, :], in_=PB(pb)[:, 0:512]), R=(pb,), W=(ht,))

        def stC(b):
            r3 = b % 3
            sl = (b // 2) % 2
            slot0 = (b // 2) * CAP + (b % 2) * 128
            ht = 'HT%d' % r3
            py = [rotC2(), rotC2()]
            for half in range(2):
                for c in range(4):
                    P.op('pe', lambda e, half=half, c=c: e.matmul(
                        T[py[half]][:, :], lhsT=T[ht][:, c * 128:(c + 1) * 128],
                        rhs=T['Wd%d' % sl][:, c * 1024 + half * 512: c * 1024 + (half + 1) * 512],
                        start=(c == 0), stop=(c == 3)), R=(ht,) + wres('Wd', sl), W=(py[half],))
            ys = 'YS%d' % r3
            P.op('act', lambda e: e.copy(out=T[ys][:, 0:512], in_=T[py[0]][:, :]), R=(py[0],), W=(ys + 'a',))
            P.op('dve', lambda e: e.tensor_copy(out=T[ys][:, 512:1024], in_=T[py[1]][:, :]), R=(py[1],), W=(ys + 'b',))
            P.op('pool', lambda e: e.indirect_dma_start(
                out=Dm['yrows'][:, :], out_offset=bass.IndirectOffsetOnAxis(ap=T['bidx'][:, b:b + 1], axis=0),
                in_=T[ys][:, :], in_offset=None, bounds_check=T['bcreg'], oob_is_err=False),
                R=(ys + 'a', ys + 'b', 'bidx'), W=(Y_RES[b], ys + 'a', ys + 'b'), dma='yst%d' % r3)

        def st0(i):
            if i >= EARLY + 1:
                for pc in pieces_at(i):
                    piece_cast(pc)
            if 0 <= i + 1 < NB:
                rowload(i + 1)
            if i + 1 >= EARLY + 1:
                for pc in pieces_at(i + 1):
                    piece_dma(pc)

        for i in range(-1, NB + 3):
            lists = [captured(st0, i)]
            if 0 <= i < NB:
                lists.append(captured(stA, i))
            if 0 <= i - 1 < NB:
                lists.append(captured(stB1, i - 1))
            if 0 <= i - 2 < NB:
                lists.append(captured(stB2, i - 2))
            if 0 <= i - 3 < NB:
                lists.append(captured(stC, i - 3))
            if MERGE_B:
                P.run_merged(lists)
            else:
                for l in lists:
                    for it in l:
                        P.op(*it)
        Y_RES = tuple(Y_RES)

        def cload(t):
            s = t % 3
            hl = 'hl%d' % s
            P.op('sp', lambda e: e.dma_start(out=T[hl][:, :], in_=Dm['h32'][t * 128:(t + 1) * 128, :]),
                 R=H32_RES, W=(hl,), dma=hl)
            for (yy, di) in (('Y1_%d' % s, 'd1i'), ('Y2_%d' % s, 'd2i')):
                if t < 3:
                    P.op('pool', lambda e, yy=yy: e.memset(T[yy][:, :], 0.0), R=(), W=(yy,))
                P.op('pool', lambda e, yy=yy, di=di: e.indirect_dma_start(
                    out=T[yy][:, :], out_offset=None, in_=Dm['yrows'][:, :],
                    in_offset=bass.IndirectOffsetOnAxis(ap=T[di][:, t:t + 1], axis=0),
                    bounds_check=T['bcreg'], oob_is_err=False), R=Y_RES + (di,), W=(yy,), dma='g' + yy)

        cload(0)
        cload(1)
        for t in range(TT):
            s = t % 3
            hl = 'hl%d' % s
            y1 = 'Y1_%d' % s
            y2 = 'Y2_%d' % s
            if t + 2 < TT:
                cload(t + 2)
            P.op('act', lambda e, hl=hl: e.mul(out=T[hl][:, :], in_=T[hl][:, :], mul=ALPHA), R=(hl,), W=(hl,))
            P.op('dve', lambda e, hl=hl, y1=y1, t=t: e.scalar_tensor_tensor(
                out=T[hl][:, :], in0=T[y1][:, :], scalar=T['W1'][:, t:t + 1], in1=T[hl][:, :], op0=ALU.mult, op1=ALU.add),
                R=(hl, y1, 'W1'), W=(hl,))
            P.op('dve', lambda e, hl=hl, y2=y2, t=t: e.scalar_tensor_tensor(
                out=T[hl][:, :], in0=T[y2][:, :], scalar=T['W2'][:, t:t + 1], in1=T[hl][:, :], op0=ALU.mult, op1=ALU.add),
                R=(hl, y2, 'W2'), W=(hl,))
            for half in range(2):
                P.op('dve', lambda e, hl=hl, half=half: e.bn_stats(out=T['lst2'][:, half * 6:(half + 1) * 6],
                                                                   in_=T[hl][:, half * 512:(half + 1) * 512]),
                     R=(hl,), W=('lst2_%d' % half,))
            P.op('dve', lambda e: e.bn_aggr(out=T['lmv2'][:, :], in_=T['lst2'][:, :]), R=('lst2_0', 'lst2_1'), W=('lmv2',))
            P.op('act', lambda e: e.activation(out=T['lsq2'][:, :], in_=T['lmv2'][:, 1:2], func=AF.Sqrt, bias=T['epsl'][:, 0:1]),
                 R=('lmv2', 'epsl'), W=('lsq2',))
            P.op('dve', lambda e, hl=hl: e.scalar_tensor_tensor(out=T[hl][:, :], in0=T[hl][:, :], scalar=T['lmv2'][:, 0:1],
                                                                in1=T['ln2g'][:, :], op0=ALU.subtract, op1=ALU.mult),
                 R=(hl, 'lmv2', 'ln2g'), W=(hl,))
            P.op('dve', lambda e: e.reciprocal(out=T['lrs2'][:, :], in_=T['lsq2'][:, :]), R=('lsq2',), W=('lrs2',))
            P.op('dve', lambda e, hl=hl: e.scalar_tensor_tensor(out=T[hl][:, :], in0=T[hl][:, :], scalar=T['lrs2'][:, 0:1],
                                                                in1=T['ln2b'][:, :], op0=ALU.mult, op1=ALU.add),
                 R=(hl, 'lrs2', 'ln2b'), W=(hl,))
            P.op('sp', lambda e, t=t, hl=hl: e.dma_start(out=Dm['out'][t * 128:(t + 1) * 128, :], in_=T[hl][:, :]),
                 R=(hl,), W=('out%d' % t,), dma='outst')


    P.barrier(engines=['sp'])
    P.analyze()

    from contextlib import ExitStack
    sem_names = ['eng_' + e for e in ENGINES] + ['dma_' + k for k in P.dma_cnt.keys()]
    with ExitStack() as cstack:
        sems = {sn: cstack.enter_context(nc.semaphore(sn)) for sn in sem_names}
        for i in range(8):
            T['bk%d' % i] = cstack.enter_context(nc.psum_tensor('bk%d' % i, [128, 512], F32))
        for name, (shape, dt, scope) in specs.items():
            if scope == 'c':
                T[name] = cstack.enter_context(nc.sbuf_tensor('s_' + name, shape, dt))

        def emit_phase(phase, scope):
            with ExitStack() as pstack:
                for name, (shape, dt, sc) in specs.items():
                    if sc == scope:
                        T[name] = pstack.enter_context(nc.sbuf_tensor('s_' + name, shape, dt))
                with nc.Block() as block:
                    def runner(engname):
                        def f(e):
                            for o in P.eng_ops[engname]:
                                if o.phase != phase:
                                    continue
                                for (sname, val) in o.waits:
                                    e.wait_ge(sems[sname], val)
                                if o.fn is not None:
                                    ins = o.fn(e)
                                    if o.is_dma:
                                        ins.then_inc(sems['dma_' + o.key], 16)
                                    elif o.signal:
                                        ins.then_inc(sems['eng_' + engname], 1)
                        return f
                    block.tensor(runner('pe'))
                    block.scalar(runner('act'))
                    block.vector(runner('dve'))
                    block.gpsimd(runner('pool'))
                    block.sync(runner('sp'))

        emit_phase(0, 'a')
        if stage >= 2:
            emit_phase(1, 'b')
    return nc


_NC_CACHE = {}


def _prep_shared(w_in, ret_gn_g, attn_sinks, w_out, ln1_g, ln1_b, w_group_router, b_group_router,
                 w_expert_router, b_expert_router, w_gate, w_up, w_down, ln2_g, ln2_b):
    f = np.float32
    sh = dict(_consts())
    w_in = np.asarray(w_in[0], f)
    cols = np.arange(INW)
    qa = np.zeros(512, np.int64)
    for j in range(4):
        for kh in range(2):
            qa[j * 128 + kh * 64: j * 128 + (kh + 1) * 64] = 2048 + kh * 256 + j * 64 + np.arange(64)
    cols[2048:2560] = qa
    sh['w_in'] = np.ascontiguousarray(w_in[:, cols])
    sh['w_out'] = np.ascontiguousarray(np.asarray(w_out[0], f))
    sh['w_gate'] = np.ascontiguousarray(np.asarray(w_gate[0], f))
    sh['w_up'] = np.ascontiguousarray(np.asarray(w_up[0], f))
    sh['w_down'] = np.ascontiguousarray(np.asarray(w_down[0], f))
    bc = lambda v: np.ascontiguousarray(np.broadcast_to(np.asarray(v, f).reshape(1, -1), (128, np.asarray(v).size)))
    sh['gng'] = bc(ret_gn_g[0])
    sk = np.zeros((128, 4), f)
    sk[0:64, :] = np.asarray(attn_sinks[0], f)[0:4][None, :]
    sk[64:128, :] = np.asarray(attn_sinks[0], f)[4:8][None, :]
    sh['sk'] = sk
    sh['sk8'] = bc(attn_sinks[0])
    sh['ln1g'] = bc(ln1_g[0])
    sh['ln1b'] = bc(ln1_b[0])
    sh['ln2g'] = bc(ln2_g[0])
    sh['ln2b'] = bc(ln2_b[0])
    wr = np.concatenate([np.asarray(w_group_router[0], f)] +
                        [np.asarray(w_expert_router[0][g], f) for g in range(4)], axis=1)
    sh['wr'] = np.ascontiguousarray(wr)
    brv = np.concatenate([np.asarray(b_group_router[0], f).reshape(-1), np.asarray(b_expert_router[0], f).reshape(-1)])
    sh['br'] = bc(brv)
    return sh


def kernel(x, w_in, ret_gn_g, attn_sinks, w_out, ln1_g, ln1_b, w_group_router, b_group_router,
           w_expert_router, b_expert_router, w_gate, w_up, w_down, ln2_g, ln2_b, _stage=2):
    if _stage not in _NC_CACHE:
        _NC_CACHE[_stage] = build(_stage)
    nc = _NC_CACHE[_stage]
    sh = _prep_shared(w_in, ret_gn_g, attn_sinks, w_out, ln1_g, ln1_b, w_group_router, b_group_router,
                      w_expert_router, b_expert_router, w_gate, w_up, w_down, ln2_g, ln2_b)
    x = np.asarray(x, np.float32)
    in_maps = []
    for c in range(8):
        m = dict(sh)
        m['x'] = np.ascontiguousarray(x[c])
        in_maps.append(m)
    res = run_bass_kernel_spmd(nc, in_maps, core_ids=list(range(8)))
    key = 'h32' if _stage == 1 else 'out'
    return np.stack([np.asarray(r[key], np.float32) for r in res.results], axis=0)
```

```python
import os
import numpy as np
import ml_dtypes
import concourse.bass as bass
import concourse.mybir as mybir
from concourse.bass_utils import run_bass_kernel_spmd

F32 = mybir.dt.float32
BF16 = mybir.dt.bfloat16
I32 = mybir.dt.int32
ALU = mybir.AluOpType
AF = mybir.ActivationFunctionType
AX = mybir.AxisListType

S = 2048
D = 1024
NT = 16
INW = 2816
NE = 32
DE = 512
CAP = 256
NSLOT = NE * CAP
ALPHA = 2.0 ** 0.25
LN_EPS = 1e-5
GN_EPS = 1e-6
BIGIDX = 1.0e6
MERGE_B = False
SPW = 12

ENGINES = ['pe', 'act', 'dve', 'pool', 'sp']


class Op:
    pass


class Prog:
    def __init__(self):
        self.ops = []
        self.eng_ops = {e: [] for e in ENGINES}
        self.last_w = {}
        self.readers = {}
        self.phase = 0
        self.dma_cnt = {}
        self.dma_last = {}
        self.capture = None

    def op(self, eng, fn, R=(), W=(), dma=None):
        if self.capture is not None:
            self.capture.append((eng, fn, tuple(R), tuple(W), dma))
            return None
        o = Op()
        o.eng = eng
        o.fn = fn
        o.phase = self.phase
        o.is_dma = dma is not None
        o.key = dma
        o.signal = False
        o.sigval = 0
        deps = []
        for r in R:
            w = self.last_w.get(r)
            if w is not None:
                deps.append((w, 'raw'))
        for r in W:
            w = self.last_w.get(r)
            if w is not None:
                deps.append((w, 'waw'))
            for rd in self.readers.get(r, {}).values():
                deps.append((rd, 'war'))
        o.deps = deps
        if o.is_dma:
            c = self.dma_cnt.get(dma, 0) + 16
            self.dma_cnt[dma] = c
            o.keyval = c
            self.dma_last[dma] = o
        for r in R:
            self.readers.setdefault(r, {})[('d', dma) if o.is_dma else ('e', eng)] = o
        for r in W:
            self.last_w[r] = o
            self.readers[r] = {}
        self.ops.append(o)
        self.eng_ops[eng].append(o)
        return o

    def run_merged(self, lists):
        lists = [l for l in lists if l]
        wts = [[(it[1] if it[0] == '__sp__' else 1) for it in l] for l in lists]
        tot = [float(sum(w)) for w in wts]
        pos = [0] * len(lists)
        used = [0.0] * len(lists)
        remaining = sum(len(l) for l in lists)
        while remaining > 0:
            best, bestf = -1, 1e9
            for k, l in enumerate(lists):
                if pos[k] < len(l):
                    f = used[k] / tot[k]
                    if f < bestf:
                        best, bestf = k, f
            it = lists[best][pos[best]]
            used[best] += wts[best][pos[best]]
            pos[best] += 1
            remaining -= 1
            if it[0] != '__sp__':
                self.op(*it)

    def spacer(self, w):
        if self.capture is not None:
            self.capture.append(('__sp__', w))

    def barrier(self, engines=ENGINES):
        lasts = []
        for e in ENGINES:
            for o in reversed(self.eng_ops[e]):
                if not o.is_dma and o.fn is not None:
                    lasts.append((o, 'raw'))
                    break
        for k, o in self.dma_last.items():
            lasts.append((o, 'raw'))
        for e in engines:
            o = Op()
            o.eng = e
            o.fn = None
            o.phase = self.phase
            o.is_dma = False
            o.key = None
            o.signal = False
            o.sigval = 0
            o.deps = list(lasts)
            o.force = True
            self.ops.append(o)
            self.eng_ops[e].append(o)

    def analyze(self):
        for e, lst in self.eng_ops.items():
            for i, o in enumerate(lst):
                o.idx = i
        waited = {e: {} for e in ENGINES}
        for o in self.ops:
            need = {}
            for (p, kind) in o.deps:
                if p.is_dma:
                    k = ('dma', p.key)
                    v = p.keyval
                else:
                    if p.eng == o.eng and not o.is_dma:
                        if getattr(o, 'force', False):
                            continue
                        if not (kind == 'raw' and o.eng in ('act', 'dve', 'pool')):
                            continue
                    k = ('eng', p.eng)
                    v = p.idx
                if waited[o.eng].get(k, -1) >= v:
                    continue
                if k not in need or need[k][0] < v:
                    need[k] = (v, p)
            o.need = need
            for k, (v, p) in need.items():
                waited[o.eng][k] = v
                if not p.is_dma:
                    p.signal = True
        for e, lst in self.eng_ops.items():
            c = 0
            for o in lst:
                if (not o.is_dma) and o.signal:
                    c += 1
                    o.sigval = c
        for o in self.ops:
            o.waits = []
            for k, (v, p) in o.need.items():
                if p.is_dma:
                    o.waits.append(('dma_' + p.key, p.keyval))
                else:
                    o.waits.append(('eng_' + p.eng, p.sigval))


def _consts():
    c = {}
    bf = ml_dtypes.bfloat16
    idx = np.arange(128, dtype=np.float64)
    c['ident'] = np.eye(128, dtype=np.float32).astype(bf)
    c['ones'] = np.ones((128, 128), np.float32).astype(bf)
    c['ustrict'] = (idx[:, None] < idx[None, :]).astype(np.float32).astype(bf)
    gam = 1.0 - 2.0 ** (-5.0 - np.arange(4, dtype=np.float64))
    m01 = (idx[:, None] <= idx[None, :]).astype(np.float32)
    c['mask01'] = np.tile(m01, (1, 4)).astype(bf)
    vs = np.zeros((128, 512), np.float64)
    rs = np.zeros((128, 512), np.float64)
    gg = np.zeros((128, 512), np.float64)
    for h in range(4):
        vs[:, h * 128:(h + 1) * 128] = (gam[h] ** (-(idx + 1.0)))[:, None]
        rs[:, h * 128:(h + 1) * 128] = (gam[h] ** (idx + 1.0))[:, None] * (128.0 ** -0.5)
        gg[:, h * 128:(h + 1) * 128] = gam[h] ** 128.0
    c['vs'] = vs.astype(np.float32)
    c['rs'] = rs.astype(np.float32)
    c['gdec'] = gg.astype(np.float32)
    slopes = 2.0 ** (-8.0 * (np.arange(8, dtype=np.float64) + 1.0) / 8.0)
    ecur = np.zeros((128, 2, 4, 128), np.float64)
    eprev = np.zeros((128, 2, 4, 128), np.float64)
    j = idx[:, None]
    i = idx[None, :]
    for kh in range(2):
        for g in range(4):
            sl = slopes[kh * 4 + g]
            ecur[:, kh, g, :] = np.where(j <= i, np.exp(-sl * (i - j)), 0.0)
            eprev[:, kh, g, :] = np.where(j > i, np.exp(-sl * (i + 128.0 - j)), 0.0)
    c['ecur'] = ecur.reshape(128, 1024).astype(np.float32).astype(bf)
    c['eprev'] = eprev.reshape(128, 1024).astype(np.float32).astype(bf)
    eb = np.tile((np.arange(NE, dtype=np.float32) * CAP)[None, :], (128, NT))
    c['ebase'] = eb.astype(np.float32)
    bb = np.arange(2 * NE)
    pp = np.arange(128)[:, None]
    c['posb'] = ((bb % 2)[None, :] * 128 + pp).astype(np.float32)
    c['slotb'] = ((bb // 2)[None, :] * CAP + (bb % 2)[None, :] * 128 + pp).astype(np.float32)
    return c


CONST_SHAPES = {
    'ident': ([128, 128], BF16), 'ones': ([128, 128], BF16), 'ustrict': ([128, 128], BF16),
    'mask01': ([128, 512], BF16), 'vs': ([128, 512], F32), 'rs': ([128, 512], F32),
    'gdec': ([128, 512], F32), 'ecur': ([128, 1024], BF16), 'eprev': ([128, 1024], BF16),
    'ebase': ([128, NT * NE], F32), 'posb': ([128, 2 * NE], F32), 'slotb': ([128, 2 * NE], F32),
}
PARAM_SHAPES = {
    'gng': ([128, 512], F32), 'sk': ([128, 4], F32), 'sk8': ([128, 8], F32), 'ln1g': ([128, D], F32), 'ln1b': ([128, D], F32),
    'ln2g': ([128, D], F32), 'ln2b': ([128, D], F32), 'br': ([128, 36], F32), 'wr': ([D, 36], F32),
}


def build(stage=2):
    nc = bass.Bass("TRN2", target_bir_lowering=False)
    Dm = {}

    def dram(name, shape, dt, kind):
        Dm[name] = nc.dram_tensor(name, shape, dt, kind=kind).ap()

    dram('x', [S, D], F32, "ExternalInput")
    dram('w_in', [D, INW], F32, "ExternalInput")
    dram('w_out', [D, D], F32, "ExternalInput")
    dram('w_gate', [NE, D, DE], F32, "ExternalInput")
    dram('w_up', [NE, D, DE], F32, "ExternalInput")
    dram('w_down', [NE, DE, D], F32, "ExternalInput")
    for k, (shp, dt) in CONST_SHAPES.items():
        dram(k, shp, dt, "ExternalInput")
    for k, (shp, dt) in PARAM_SHAPES.items():
        dram(k, shp, dt, "ExternalInput")
    dram('out', [S, D], F32, "ExternalOutput")
    if stage == 1:
        dram('h32', [S, D], F32, "ExternalOutput")
    else:
        dram('h32', [S, D], F32, "Internal")
    dram('rows', [NSLOT, D], BF16, "Internal")
    dram('yrows', [NSLOT, D], F32, "Internal")

    P = Prog()
    T = {}

    specs = {}

    def sb(name, shape, dt, scope):
        specs[name] = (shape, dt, scope)

    NSTG = 5
    NX = 4
    for i in range(NSTG):
        sb('stg%d' % i, [128, 2048], F32, 'b')
    for i in range(3):
        sb('stgA%d' % i, [128, 2048], F32, 'a')
    for i in range(2):
        sb('Wg%d' % i, [128, 4096], BF16, 'b')
        sb('Wu%d' % i, [128, 4096], BF16, 'b')
        sb('Wd%d' % i, [128, 4096], BF16, 'b')
    sb('L', [128, NT * 36], F32, 'c')
    sb('ident', [128, 128], BF16, 'c')
    sb('ones', [128, 128], BF16, 'c')

    psf_i = [0]
    psb_i = [0]

    def next_psf():
        psf_i[0] = (psf_i[0] + 1) % 2
        return 'bk%d' % (6 + psf_i[0])

    def next_psb():
        psb_i[0] = (psb_i[0] + 1) % 2
        return 'psb%d' % psb_i[0]

    def make_rot(names):
        st = [0]

        def f():
            st[0] = (st[0] + 1) % len(names)
            return names[st[0]]
        return f

    rotB1 = make_rot(['bk0', 'bk1'])
    rotC2 = make_rot(['bk2', 'bk3'])

    def load_const(name, dst=None, eng='sp'):
        dst = dst or name
        P.op(eng, lambda e, n=name, d=dst: e.dma_start(out=T[d][:, :], in_=Dm[n][:, :]),
             R=(), W=(dst,), dma='c_' + dst)

    P.phase = 0
    sb('Win', [128, 8 * INW], BF16, 'a')
    sb('Wout', [128, 8 * D], BF16, 'a')
    sb('Wr', [128, 8 * 36], BF16, 'a')
    sb('Wrf', [128, 8 * 36], F32, 'a')
    for i in range(2):
        sb('xin%d' % i, [128, D], F32, 'a')
        sb('xb%d' % i, [128, D], BF16, 'a')
        sb('xT%d' % i, [128, D], BF16, 'a')
        sb('stm%d' % i, [128, 512], BF16, 'a')
        sb('orb%d' % i, [128, 512], BF16, 'a')
        sb('orT%d' % i, [128, D], BF16, 'a')
        sb('hb%d' % i, [128, D], BF16, 'a')
        sb('hT%d' % i, [128, D], BF16, 'a')
        for kh in range(2):
            sb('Pc%d_%d' % (kh, i), [128, 512], BF16, 'a')
            sb('Pp%d_%d' % (kh, i), [128, 512], BF16, 'a')
    for i in range(3):
        sb('xr%d' % i, [128, D], F32, 'a')
        sb('fm%d' % i, [128, 12 * 128], BF16, 'a')
        sb('kTa%d' % i, [128, 128], BF16, 'a')
        sb('kr%d' % i, [128, 512], BF16, 'a')
        sb('vp%d' % i, [128, 512], BF16, 'a')
        sb('tg%d' % i, [128, 512], F32, 'a')
    for i in range(4):
        sb('va%d' % i, [128, 130], BF16, 'a')
    sb('sk8', [128, 8], F32, 'a')
    sb('EST', [128, 8], F32, 'a')
    sb('dnt', [128, 8], F32, 'a')
    sb('rdt', [128, 8], F32, 'a')
    for i in range(2):
        sb('oab%d' % i, [128, 512], BF16, 'a')
    sb('stbf', [128, 512], BF16, 'a')
    for n_ in ('st32', 'tmp32', 'os', 'yn'):
        sb(n_, [128, 512], F32, 'a')
    sb('gst', [128, 24], F32, 'a')
    sb('gmv', [128, 8], F32, 'a')
    sb('grs', [128, 4], F32, 'a')
    sb('gnb', [128, 4], F32, 'a')
    sb('lst', [128, 12], F32, 'a')
    sb('lmv', [128, 2], F32, 'a')
    sb('lrs', [128, 1], F32, 'a')
    sb('lnb', [128, 1], F32, 'a')
    sb('gsq', [128, 4], F32, 'a')
    sb('lsq', [128, 1], F32, 'a')
    sb('epsg', [128, 1], F32, 'c')
    sb('epsl', [128, 1], F32, 'c')
    sb('thb', [128, 512], F32, 'a')
    for k in ('mask01', 'ecur', 'eprev'):
        sb(k, CONST_SHAPES[k][0], BF16, 'a')
    for k in ('vs', 'rs', 'gdec'):
        sb(k, [128, 512], F32, 'a')
    sb('gng', [128, 512], F32, 'a')
    sb('sk', [128, 4], F32, 'a')
    sb('ske', [128, 4], F32, 'a')
    sb('ln1g', [128, D], F32, 'a')
    sb('ln1b', [128, D], F32, 'a')
    sb('br', [128, 36], F32, 'a')

    load_const('ident')
    P.op('sp', lambda e: e.dma_start(out=T['xin0'][:, :], in_=Dm['x'][0:128, :]), R=(), W=('xin0',), dma='xin0')

    P.op('pool', lambda e: e.memset(T['epsg'][:, :], 4.0 * GN_EPS), R=(), W=('epsg',))
    P.op('pool', lambda e: e.memset(T['epsl'][:, :], LN_EPS), R=(), W=('epsl',))

    cast_rr = [0]

    def cast_op(dst_fn, src_fn, R, W, eng=None):
        if eng is None:
            eng = ('act', 'dve')[cast_rr[0] % 2]
            cast_rr[0] += 1
        if eng == 'act':
            P.op('act', lambda e: e.copy(out=dst_fn(), in_=src_fn()), R=R, W=W)
        elif eng == 'dve':
            P.op('dve', lambda e: e.tensor_copy(out=dst_fn(), in_=src_fn()), R=R, W=W)
        else:
            P.op('pool', lambda e: e.tensor_copy(out=dst_fn(), in_=src_fn()), R=R, W=W)

    stg_i = [0]

    stgA_i = [0]

    def next_stgA():
        s = stgA_i[0] % 3
        stgA_i[0] += 1
        return s

    def next_stg():
        s = stg_i[0] % NSTG
        stg_i[0] += 1
        return s

    HW = INW // 2
    for hh in range(2):
        for kc in range(8):
            s = next_stgA()
            P.op('sp', lambda e, kc=kc, hh=hh, s=s: e.dma_start(
                out=T['stgA%d' % s][:, 0:HW], in_=Dm['w_in'][kc * 128:(kc + 1) * 128, hh * HW:(hh + 1) * HW]),
                R=(), W=('stgA%d' % s,), dma='stgA%d' % s)
            cast_op(lambda kc=kc, hh=hh: T['Win'][:, kc * INW + hh * HW: kc * INW + (hh + 1) * HW],
                    lambda s=s: T['stgA%d' % s][:, 0:HW],
                    R=('stgA%d' % s,), W=('Win%d_%d' % (kc, hh),))
    WIN_RES = tuple('Win%d_%d' % (kc, hh) for kc in range(8) for hh in range(2))

    def win_res(c0, c1):
        hs = set()
        if c0 < HW:
            hs.add(0)
        if c1 > HW:
            hs.add(1)
        return tuple('Win%d_%d' % (kc, hh) for kc in range(8) for hh in sorted(hs))
    for k in ('ones', 'mask01', 'vs', 'rs', 'gdec', 'ecur', 'eprev', 'gng', 'sk8', 'br', 'ln1g', 'ln1b'):
        load_const(k)
    wout_slots = []
    for q in range(4):
        s_ = next_stgA()
        wout_slots.append(s_)
        if q < 3:
            P.op('sp', lambda e, q=q, s_=s_: e.dma_start(
                out=T['stgA%d' % s_][:, :].rearrange("p (c n) -> p c n", c=2),
                in_=Dm['w_out'][q * 256:(q + 1) * 256, :].rearrange("(c p) n -> p c n", p=128)),
                R=(), W=('stgA%d' % s_,), dma='stgA%d' % s_)

    def wout_casts():
        for q in range(4):
            s_ = wout_slots[q]
            if q == 3:
                P.op('sp', lambda e, q=q, s_=s_: e.dma_start(
                    out=T['stgA%d' % s_][:, :].rearrange("p (c n) -> p c n", c=2),
                    in_=Dm['w_out'][q * 256:(q + 1) * 256, :].rearrange("(c p) n -> p c n", p=128)),
                    R=(), W=('stgA%d' % s_,), dma='stgA%d' % s_)
            cast_op(lambda q=q: T['Wout'][:, q * 2048:(q + 1) * 2048], lambda s_=s_: T['stgA%d' % s_][:, :],
                    R=('stgA%d' % s_,), W=('Wout%d' % q,))
    WOUT_RES = tuple('Wout%d' % q for q in range(4))
    P.op('sp', lambda e: e.dma_start(out=T['Wrf'][:, :].rearrange("p (c n) -> p c n", c=8),
                                     in_=Dm['wr'][:, :].rearrange("(c p) n -> p c n", p=128)),
         R=(), W=('Wrf',), dma='c_Wrf')
    P.op('dve', lambda e: e.tensor_copy(out=T['Wr'][:, :], in_=T['Wrf'][:, :]), R=('Wrf',), W=('Wr',))
    P.op('act', lambda e: e.activation(out=T['EST'][:, :], in_=T['sk8'][:, :], func=AF.Exp), R=('sk8',), W=('EST',))
    for i in range(4):
        P.op('pool', lambda e, i=i: e.memset(T['va%d' % i][:, :], 1.0), R=(), W=('va%d' % i,))

    sb('zt', [128, 2048], BF16, 'a')
    P.op('pool', lambda e: e.memset(T['zt'][:, :], 0.0), R=(), W=('zt',))
    def zero_dma(q):
        P.op('pool', lambda e, q=q: e.dma_start(
            out=Dm['rows'][q * 256:(q + 1) * 256, :].rearrange("(p r) f -> p (r f)", p=128),
            in_=T['zt'][:, :]), R=('zt',), W=('rowsz%d' % q,), dma='zero')
    ROWSZ = tuple('rowsz%d' % q for q in range(32))

    def PB(bank):
        return T[bank][:, :].bitcast(BF16)

    def xload(n):
        s = n % 2
        P.op('sp', lambda e: e.dma_start(out=T['xin%d' % s][:, :], in_=Dm['x'][n * 128:(n + 1) * 128, :]),
             R=(), W=('xin%d' % s,), dma='xin%d' % s)

    def xrload(n):
        s = n % 3
        P.op('sp', lambda e: e.dma_start(out=T['xr%d' % s][:, :], in_=Dm['x'][n * 128:(n + 1) * 128, :]),
             R=(), W=('xr%d' % s,), dma='xr%d' % s)

    def T0(n):
        if n + 1 < NT:
            xload(n + 1)
        xin = 'xin%d' % (n % 2)
        xb = 'xb%d' % (n % 2)
        P.op('act', lambda e: e.copy(out=T[xb][:, :], in_=T[xin][:, :]), R=(xin,), W=(xb,))
        if n == 1:
            wout_casts()

    def T1(n):
        xb = 'xb%d' % (n % 2)
        xT = 'xT%d' % (n % 2)
        bk = 'bk0'
        for kc in range(8):
            P.op('pe', lambda e, kc=kc: e.transpose(out=PB(bk)[:, kc * 128:(kc + 1) * 128],
                                                    in_=T[xb][:, kc * 128:(kc + 1) * 128],
                                                    identity=T['ident'][:, :]),
                 R=(xb, 'ident'), W=(bk,))
        P.op('dve', lambda e: e.tensor_copy(out=T[xT][:, :], in_=PB(bk)[:, :]), R=(bk,), W=(xT,))

    rot2 = make_rot(['bk1', 'bk2'])
    rot4 = make_rot(['bk4', 'bk5'])

    def T2(n):
        s2 = n % 2
        s3 = n % 3
        xT = 'xT%d' % s2
        fm = 'fm%d' % s3
        kTa = 'kTa%d' % s3
        va = 'va%d' % (n % 4)
        kr = 'kr%d' % s3
        vp = 'vp%d' % s3
        tg = 'tg%d' % s3

        def fm_group(cols0, nch, evac_eng, dst_fn, dst_res):
            pf = rot2()
            for c in range(nch):
                for kc in range(8):
                    P.op('pe', lambda e, c=c, kc=kc: e.matmul(
                        T[pf][:, c * 128:(c + 1) * 128],
                        lhsT=T['Win'][:, kc * INW + cols0 + c * 128: kc * INW + cols0 + (c + 1) * 128],
                        rhs=T[xT][:, kc * 128:(kc + 1) * 128], start=(kc == 0), stop=(kc == 7)),
                        R=win_res(cols0, cols0 + nch * 128) + (xT,), W=(pf,))
            if evac_eng == 'act':
                P.op('act', lambda e: e.copy(out=dst_fn(), in_=T[pf][:, 0:nch * 128]), R=(pf,), W=(dst_res,))
            else:
                P.op('dve', lambda e: e.tensor_copy(out=dst_fn(), in_=T[pf][:, 0:nch * 128]), R=(pf,), W=(dst_res,))

        def tm_group(cols0, ncols):
            pf = rot2()
            for kc in range(8):
                P.op('pe', lambda e, kc=kc: e.matmul(
                    T[pf][:, 0:ncols], lhsT=T[xT][:, kc * 128:(kc + 1) * 128],
                    rhs=T['Win'][:, kc * INW + cols0: kc * INW + cols0 + ncols], start=(kc == 0), stop=(kc == 7)),
                    R=win_res(cols0, cols0 + ncols) + (xT,), W=(pf,))
            return pf

        fm_group(0, 4, 'act', lambda: T[fm][:, 0:512], fm + '_q')
        fm_group(512, 4, 'dve', lambda: T[fm][:, 512:1024], fm + '_k')
        pf1 = tm_group(512, 512)
        P.op('act', lambda e: e.copy(out=T[kr][:, :], in_=T[pf1][:, :]), R=(pf1,), W=(kr,))
        fm_group(2048, 4, 'act', lambda: T[fm][:, 1024:1536], fm + '_qa')
        pf2 = tm_group(1024, 512)
        P.op('dve', lambda e: e.tensor_tensor(out=T[vp][:, :], in0=T[pf2][:, :], in1=T['vs'][:, :], op=ALU.mult),
             R=(pf2, 'vs'), W=(vp,))
        fm_group(2560, 1, 'dve', lambda: T[kTa][:, :], kTa)
        pf3 = tm_group(1536, 512)
        P.op('act', lambda e: e.activation(out=T['thb'][:, :], in_=T[pf3][:, :], func=AF.Tanh, scale=0.5), R=(pf3,), W=('thb',))
        pf4 = tm_group(2688, 128)
        P.op('dve', lambda e: e.scalar_tensor_tensor(out=T['thb'][:, :], in0=T['thb'][:, :], scalar=1.0, in1=T[pf3][:, :],
                                                     op0=ALU.add, op1=ALU.mult), R=(pf3, 'thb'), W=('thb',))
        P.op('dve', lambda e: e.tensor_copy(out=T[va][:, :].rearrange("p (k c) -> p k c", k=2)[:, :, 0:64],
                                            in_=T[pf4][:, 0:128].rearrange("p (k c) -> p k c", k=2)), R=(pf4,), W=(va,))
        P.op('pool', lambda e: e.tensor_tensor(out=T[tg][:, :], in0=T['thb'][:, :], in1=T['gng'][:, :], op=ALU.mult),
             R=('thb', 'gng'), W=(tg,))

    def T3(n):
        s2 = n % 2
        s3 = n % 3
        p3 = (n - 1) % 3
        fm = 'fm%d' % s3
        kTa = 'kTa%d' % s3
        stm = 'stm%d' % s2
        bk = 'bk3'
        for h in range(4):
            P.op('pe', lambda e, h=h: e.matmul(
                T[bk][:, h * 128:(h + 1) * 128], lhsT=T[fm][:, 512 + h * 128: 512 + (h + 1) * 128],
                rhs=T[fm][:, h * 128:(h + 1) * 128], start=True, stop=True),
                R=(fm + '_q', fm + '_k'), W=(bk,))
        P.op('dve', lambda e: e.tensor_tensor(out=T[stm][:, :], in0=T[bk][:, :], in1=T['mask01'][:, :], op=ALU.mult),
             R=(bk, 'mask01'), W=(stm,))
        for kh in range(2):
            for which in ('c', 'p'):
                if which == 'p' and n == 0:
                    continue
                kt = kTa if which == 'c' else 'kTa%d' % p3
                P.op('pe', lambda e, kh=kh, kt=kt: e.matmul(
                    T[bk][:, :], lhsT=T[kt][kh * 64:(kh + 1) * 64, :],
                    rhs=T[fm][kh * 64:(kh + 1) * 64, 1024:1536], start=True, stop=True),
                    R=(kt, fm + '_qa'), W=(bk,))
                pn = 'P%s%d_%d' % (which, kh, s2)
                et = 'ecur' if which == 'c' else 'eprev'
                P.op('act', lambda e, pn=pn: e.activation(out=T[pn][:, :], in_=T[bk][:, :], func=AF.Exp, scale=0.125),
                     R=(bk,), W=(pn,))
                P.op('pool', lambda e, pn=pn, et=et, kh=kh: e.tensor_tensor(
                    out=T[pn][:, :], in0=T[pn][:, :], in1=T[et][:, kh * 512:(kh + 1) * 512], op=ALU.mult),
                    R=(pn, et), W=(pn,))

    def T4(n):
        s2 = n % 2
        s3 = n % 3
        fm = 'fm%d' % s3
        va = 'va%d' % (n % 4)
        vap = 'va%d' % ((n - 1) % 4)
        kr = 'kr%d' % s3
        vp = 'vp%d' % s3
        tg = 'tg%d' % s3
        stm = 'stm%d' % s2
        orb = 'orb%d' % s2
        po = rot4()
        for h in range(4):
            P.op('pe', lambda e, h=h: e.matmul(
                T[po][:, h * 128:(h + 1) * 128], lhsT=T[stm][:, h * 128:(h + 1) * 128],
                rhs=T[vp][:, h * 128:(h + 1) * 128], start=True, stop=(n == 0)),
                R=(stm, vp), W=(po,))
            if n > 0:
                P.op('pe', lambda e, h=h: e.matmul(
                    T[po][:, h * 128:(h + 1) * 128], lhsT=T[fm][:, h * 128:(h + 1) * 128],
                    rhs=T['stbf'][:, h * 128:(h + 1) * 128], start=False, stop=True),
                    R=(fm + '_q', 'stbf'), W=(po,))
        pkv = rot4()
        for h in range(4):
            P.op('pe', lambda e, h=h: e.matmul(
                T[pkv][:, h * 128:(h + 1) * 128], lhsT=T[kr][:, h * 128:(h + 1) * 128],
                rhs=T[vp][:, h * 128:(h + 1) * 128], start=True, stop=True),
                R=(kr, vp), W=(pkv,))
        if n == 0:
            P.op('dve', lambda e: e.tensor_tensor(out=T['st32'][:, :], in0=T[pkv][:, :], in1=T['gdec'][:, :], op=ALU.mult),
                 R=(pkv, 'gdec'), W=('st32',))
        else:
            P.op('dve', lambda e: e.tensor_tensor(out=T['tmp32'][:, :], in0=T[pkv][:, :], in1=T['st32'][:, :], op=ALU.add),
                 R=(pkv, 'st32'), W=('tmp32',))
            P.op('pool', lambda e: e.tensor_tensor(out=T['st32'][:, :], in0=T['tmp32'][:, :], in1=T['gdec'][:, :], op=ALU.mult),
                 R=('tmp32', 'gdec'), W=('st32',))
        if n + 1 < NT:
            P.op('act', lambda e: e.copy(out=T['stbf'][:, :], in_=T['st32'][:, :]), R=('st32',), W=('stbf',))
        P.op('dve', lambda e: e.tensor_tensor(out=T['os'][:, :], in0=T[po][:, :], in1=T['rs'][:, :], op=ALU.mult),
             R=(po, 'rs'), W=('os',))
        oab = 'oab%d' % s2
        for kh in range(2):
            pb_ = rot4()
            seq = ([('p', vap)] if n > 0 else []) + [('c', va)]
            for g in range(4):
                for idx_, (which, vt) in enumerate(seq):
                    pn = 'P%s%d_%d' % (which, kh, s2)
                    P.op('pe', lambda e, pn=pn, vt=vt, pb_=pb_, g=g, kh=kh, first=(idx_ == 0), last=(idx_ == len(seq) - 1): e.matmul(
                        T[pb_][:, g * 65:(g + 1) * 65], lhsT=T[pn][:, g * 128:(g + 1) * 128],
                        rhs=T[vt][:, kh * 65:(kh + 1) * 65], start=first, stop=last),
                        R=(pn, vt), W=(pb_,))
            oav = lambda pb_=pb_: T[pb_][:, 0:260].rearrange("p (g c) -> p g c", g=4)
            P.op('dve', lambda e, oav=oav, kh=kh: e.tensor_tensor(
                out=T['dnt'][:, kh * 4:(kh + 1) * 4], in0=oav()[:, :, 64], in1=T['EST'][:, kh * 4:(kh + 1) * 4], op=ALU.add),
                R=(pb_, 'EST'), W=('dnt%d' % kh,))
            P.op('dve', lambda e, kh=kh: e.reciprocal(out=T['rdt'][:, kh * 4:(kh + 1) * 4], in_=T['dnt'][:, kh * 4:(kh + 1) * 4]),
                 R=('dnt%d' % kh,), W=('rdt%d' % kh,))
            P.op('dve', lambda e, oav=oav, kh=kh: e.tensor_tensor(
                out=T[oab][:, kh * 256:(kh + 1) * 256].rearrange("p (g c) -> p g c", g=4), in0=oav()[:, :, 0:64],
                in1=T['rdt'][:, kh * 4:(kh + 1) * 4].unsqueeze(2).to_broadcast([128, 4, 64]), op=ALU.mult),
                R=(pb_, 'rdt%d' % kh), W=(oab + '_%d' % kh,))
        for h in range(4):
            P.op('dve', lambda e, h=h: e.bn_stats(out=T['gst'][:, h * 6:(h + 1) * 6], in_=T['os'][:, h * 128:(h + 1) * 128]),
                 R=('os',), W=('gst%d' % h,))
        for h in range(4):
            P.op('dve', lambda e, h=h: e.bn_aggr(out=T['gmv'][:, h * 2:(h + 1) * 2], in_=T['gst'][:, h * 6:(h + 1) * 6]),
                 R=('gst%d' % h,), W=('gmv%d' % h,))
        gmv_v = lambda: T['gmv'][:, :].rearrange("p (h s) -> p h s", h=4)
        GMV = ('gmv0', 'gmv1', 'gmv2', 'gmv3')
        P.spacer(SPW)
        P.op('act', lambda e: e.activation(out=T['gsq'][:, :], in_=gmv_v()[:, :, 1], func=AF.Sqrt, scale=4.0, bias=T['epsg'][:, 0:1]),
             R=GMV + ('epsg',), W=('gsq',))
        P.spacer(SPW)
        P.op('dve', lambda e: e.reciprocal(out=T['grs'][:, :], in_=T['gsq'][:, :]), R=('gsq',), W=('grs',))
        P.op('dve', lambda e: e.scalar_tensor_tensor(out=T['gnb'][:, :], in0=gmv_v()[:, :, 0], scalar=-1.0, in1=T['grs'][:, :],
                                                     op0=ALU.mult, op1=ALU.mult), R=GMV + ('grs',), W=('gnb',))
        P.spacer(SPW)
        for h in range(4):
            P.op('act', lambda e, h=h: e.activation(out=T['yn'][:, h * 128:(h + 1) * 128], in_=T['os'][:, h * 128:(h + 1) * 128],
                                                    func=AF.Identity, scale=T['grs'][:, h:h + 1], bias=T['gnb'][:, h:h + 1]),
                 R=('os', 'grs', 'gnb'), W=('yn',))
        P.spacer(SPW)
        P.op('pool', lambda e: e.tensor_tensor(out=T[orb][:, :], in0=T['yn'][:, :], in1=T[tg][:, :], op=ALU.mult),
             R=('yn', tg), W=(orb,))

    def T5(n):
        orb = 'orb%d' % (n % 2)
        oab = 'oab%d' % (n % 2)
        orT = 'orT%d' % (n % 2)
        bk = 'bk0'
        for h in range(4):
            P.op('pe', lambda e, h=h: e.transpose(out=PB(bk)[:, h * 128:(h + 1) * 128], in_=T[orb][:, h * 128:(h + 1) * 128],
                                                  identity=T['ident'][:, :]), R=(orb, 'ident'), W=(bk,))
        for c in range(4):
            P.op('pe', lambda e, c=c: e.transpose(out=PB(bk)[:, (4 + c) * 128:(5 + c) * 128], in_=T[oab][:, c * 128:(c + 1) * 128],
                                                  identity=T['ident'][:, :]), R=(oab + '_0', oab + '_1', 'ident'), W=(bk,))
        P.op('act', lambda e: e.copy(out=T[orT][:, :], in_=PB(bk)[:, :]), R=(bk,), W=(orT,))
        xrload(n)

    def T6(n):
        xin = 'xr%d' % (n % 3)
        orT = 'orT%d' % (n % 2)
        hb = 'hb%d' % (n % 2)
        bks = ['bk6', 'bk7']
        for half in range(2):
            for kc in range(8):
                src = (lambda kc=kc: T[orT][:, kc * 128:(kc + 1) * 128])
                P.op('pe', lambda e, half=half, kc=kc, src=src: e.matmul(
                    T[bks[half]][:, :], lhsT=src(), rhs=T['Wout'][:, kc * D + half * 512: kc * D + (half + 1) * 512],
                    start=(kc == 0), stop=(kc == 7)),
                    R=WOUT_RES + (orT,), W=(bks[half],))
            P.op('dve', lambda e, half=half: e.scalar_tensor_tensor(
                out=T[xin][:, half * 512:(half + 1) * 512], in0=T[xin][:, half * 512:(half + 1) * 512], scalar=ALPHA,
                in1=T[bks[half]][:, :], op0=ALU.mult, op1=ALU.add), R=(bks[half], xin), W=(xin + 'h%d' % half,))
        for half in range(2):
            P.op('dve', lambda e, half=half: e.bn_stats(out=T['lst'][:, half * 6:(half + 1) * 6],
                                                        in_=T[xin][:, half * 512:(half + 1) * 512]),
                 R=(xin + 'h%d' % half,), W=('lst%d' % half,))
        P.op('dve', lambda e: e.bn_aggr(out=T['lmv'][:, :], in_=T['lst'][:, :]), R=('lst0', 'lst1'), W=('lmv',))
        P.op('dve', lambda e: e.scalar_tensor_tensor(out=T[xin][:, :], in0=T[xin][:, :], scalar=T['lmv'][:, 0:1],
                                                     in1=T['ln1g'][:, :], op0=ALU.subtract, op1=ALU.mult),
             R=(xin, xin + 'h0', xin + 'h1', 'lmv', 'ln1g'), W=(xin, xin + 'h0', xin + 'h1'))
        P.spacer(SPW)
        P.op('act', lambda e: e.activation(out=T['lsq'][:, :], in_=T['lmv'][:, 1:2], func=AF.Sqrt, bias=T['epsl'][:, 0:1]),
             R=('lmv', 'epsl'), W=('lsq',))
        P.spacer(SPW)
        P.op('dve', lambda e: e.reciprocal(out=T['lrs'][:, :], in_=T['lsq'][:, :]), R=('lsq',), W=('lrs',))
        P.op('dve', lambda e: e.scalar_tensor_tensor(out=T[xin][:, :], in0=T[xin][:, :], scalar=T['lrs'][:, 0:1],
                                                     in1=T['ln1b'][:, :], op0=ALU.mult, op1=ALU.add),
             R=(xin, 'lrs', 'ln1b'), W=(xin,))
        P.spacer(SPW)
        P.op('sp', lambda e: e.dma_start(out=Dm['h32'][n * 128:(n + 1) * 128, :], in_=T[xin][:, :]),
             R=(xin,), W=('h32_%d' % n,), dma='h32st')
        P.op('act', lambda e: e.copy(out=T[hb][:, :], in_=T[xin][:, :]), R=(xin,), W=(hb,))

    def T7(n):
        hb = 'hb%d' % (n % 2)
        hT = 'hT%d' % (n % 2)
        bk = 'bk0'
        for kc in range(8):
            P.op('pe', lambda e, kc=kc: e.transpose(out=PB(bk)[:, kc * 128:(kc + 1) * 128], in_=T[hb][:, kc * 128:(kc + 1) * 128],
                                                    identity=T['ident'][:, :]), R=(hb, 'ident'), W=(bk,))
        P.op('dve', lambda e: e.tensor_copy(out=T[hT][:, :], in_=PB(bk)[:, :]), R=(bk,), W=(hT,))

    def T8(n):
        hT = 'hT%d' % (n % 2)
        bk = 'bk0'
        for kc in range(8):
            P.op('pe', lambda e, kc=kc: e.matmul(T[bk][:, 0:36], lhsT=T[hT][:, kc * 128:(kc + 1) * 128],
                                                 rhs=T['Wr'][:, kc * 36:(kc + 1) * 36], start=(kc == 0), stop=(kc == 7)),
                 R=(hT, 'Wr'), W=(bk,))
        P.op('dve', lambda e: e.tensor_tensor(out=T['L'][:, n * 36:(n + 1) * 36], in0=T[bk][:, 0:36], in1=T['br'][:, :], op=ALU.add),
             R=(bk, 'br'), W=('L%d' % n,))

    def captured(fn, *a):
        P.capture = []
        fn(*a)
        l = P.capture
        P.capture = None
        return l

    GROUPS = [[(T0, 0)], [(T1, 1), (T5, 5), (T7, 7), (T8, 8)], [(T2, 2)], [(T3, 3)], [(T4, 4)], [(T6, 6)]]

    def run_group(grp, i):
        for (st, k) in grp:
            if 0 <= i - k < NT:
                st(i - k)

    for i in range(NT + 8):
        lists = [captured(run_group, grp, i) for grp in GROUPS]
        P.run_merged(lists)

    H32_RES = tuple('h32_%d' % n for n in range(NT))
    L_RES = tuple('L%d' % n for n in range(NT))

    if stage >= 2:
        P.barrier()
        P.phase = 1
        TT = NT
        sb('ustrict', [128, 128], BF16, 'b')
        sb('ebase', [128, NT * NE], F32, 'b')
        sb('ln2g', [128, D], F32, 'b')
        sb('ln2b', [128, D], F32, 'b')
        sb('posb', [128, 2 * NE], F32, 'b')
        sb('slotb', [128, 2 * NE], F32, 'b')
        sb('cnt', [128, NE], F32, 'b')
        sb('bovf', [128, 2 * NE], F32, 'b')
        sb('bidx', [128, 2 * NE], I32, 'b')
        for k in ('ustrict', 'ebase', 'ln2g', 'ln2b', 'posb', 'slotb'):
            load_const(k)
        for i in range(3):
            P.op('pool', lambda e, i=i: e.memset(T['RW%d' % i][:, :], 0.0), R=(), W=('RW%d' % i,))

        def mkreg(e):
            T['bcreg'] = e.alloc_register("bcreg")
            return e.reg_mov(T['bcreg'], NSLOT - 1)
        P.op('pool', mkreg)

        WBN = {'w_gate': 'Wg', 'w_up': 'Wu', 'w_down': 'Wd'}

        def pieces_at(st):
            out = []
            if st % 2 != 0:
                e1 = (st + 1) // 2
                if 0 <= e1 < NE:
                    out += [(e1, 'w_gate', 0, 'act'), (e1, 'w_up', 0, 'dve')]
                e2 = (st - 1) // 2
                if 0 <= e2 < NE:
                    out += [(e2, 'w_down', 0, 'dve')]
            else:
                e1 = st // 2
                if 0 <= e1 < NE:
                    out += [(e1, 'w_gate', 1, 'act'), (e1, 'w_up', 1, 'dve')]
                e2 = st // 2 - 1
                if 0 <= e2 < NE:
                    out += [(e2, 'w_down', 1, 'dve')]
            return out

        piece_slot = {}

        def piece_dma(pc):
            ex, nm, hh, _ = pc
            s = next_stg()
            piece_slot[pc] = s
            if nm == 'w_down':
                P.op('sp', lambda e: e.dma_start(
                    out=T['stg%d' % s][:, :].rearrange("p (c n) -> p c n", c=2),
                    in_=Dm[nm][ex, hh * 256:(hh + 1) * 256, :].rearrange("(c p) n -> p c n", p=128)),
                    R=(), W=('stg%d' % s,), dma='stg%d' % s)
            else:
                P.op('sp', lambda e: e.dma_start(
                    out=T['stg%d' % s][:, :].rearrange("p (c n) -> p c n", c=4),
                    in_=Dm[nm][ex, hh * 512:(hh + 1) * 512, :].rearrange("(c p) n -> p c n", p=128)),
                    R=(), W=('stg%d' % s,), dma='stg%d' % s)

        def piece_cast(pc):
            ex, nm, hh, eng = pc
            s = piece_slot[pc]
            wb = WBN[nm]
            sl = ex % 2
            cast_op(lambda: T['%s%d' % (wb, sl)][:, hh * 2048:(hh + 1) * 2048], lambda: T['stg%d' % s][:, :],
                    R=('stg%d' % s,), W=('%s%d_%d' % (wb, sl, hh),), eng=eng)


        def hload(t):
            s = t % 3
            P.op('sp', lambda e: e.dma_start(out=T['hl%d' % s][:, :], in_=Dm['h32'][t * 128:(t + 1) * 128, :]),
                 R=H32_RES, W=('hl%d' % s,), dma='hl%d' % s)

        hload(0)
        hload(1)

        EARLY = 2
        _early = [pc for st_ in range(-1, EARLY + 1) for pc in pieces_at(st_)]
        _issued = [0]
        _casted = [0]

        def early_step():
            if _casted[0] < len(_early):
                piece_cast(_early[_casted[0]])
                _casted[0] += 1
            if _issued[0] < len(_early):
                piece_dma(_early[_issued[0]])
                _issued[0] += 1

        for _ in range(NSTG):
            piece_dma(_early[_issued[0]])
            _issued[0] += 1
        for n_, w in (('gmax', TT), ('goh', TT * 4), ('gsh', TT * 4), ('gex', TT * 4), ('gsum', TT), ('gw', TT),
                      ('elm', TT * 32), ('els', TT * 8), ('m1', TT), ('oh1', TT * 8), ('els2', TT * 8), ('m2', TT),
                      ('oh2', TT * 8), ('dd', TT), ('ed', TT), ('ed1', TT), ('w1', TT), ('W1', TT), ('W2', TT),
                      ('A1', TT * 32), ('A2', TT * 32), ('Asum', TT * 32), ('CT', TT * 32), ('rank', TT * 32),
                      ('ovf', TT * 32), ('pos', TT * 32), ('pm1', TT * 32), ('d1f', TT), ('d2f', TT)):
            sb(n_, [128, w], F32, 'b')
        sb('Abf', [128, TT * 32], BF16, 'b')
        sb('d1i', [128, TT], I32, 'b')
        sb('d2i', [128, TT], I32, 'b')
        for i in range(3):
            sb('hbb%d' % i, [128, D], BF16, 'b')
        for i in range(2):
            sb('sgf%d' % i, [128, 512], F32, 'b')
        for i in range(3):
            sb('hl%d' % i, [128, D], F32, 'b')
            sb('XT%d' % i, [128, D], BF16, 'b')
            sb('Hh%d' % i, [128, 512], BF16, 'b')
            sb('HT%d' % i, [128, 512], BF16, 'b')
            sb('YS%d' % i, [128, D], F32, 'b')
            sb('Y1_%d' % i, [128, D], F32, 'b')
            sb('Y2_%d' % i, [128, D], F32, 'b')
            sb('RW%d' % i, [128, D], BF16, 'b')
        sb('lst2', [128, 12], F32, 'b')
        sb('lmv2', [128, 2], F32, 'b')
        sb('lrs2', [128, 1], F32, 'b')
        sb('lnb2', [128, 1], F32, 'b')
        sb('lsq2', [128, 1], F32, 'b')

        def v3(name, a, b):
            return T[name][:, :].rearrange("p (a b) -> p a b", a=a)

        def v4(name, a, b, c):
            return T[name][:, :].rearrange("p (a b c) -> p a b c", a=a, b=b)

        Lv = lambda: T['L'][:, :].rearrange("p (t c) -> p t c", t=TT)
        gl = lambda: Lv()[:, :, 0:4]
        el = lambda: Lv()[:, :, 4:36].rearrange("p t (g e) -> p t g e", g=4)

        def dve(fn, R, W):
            P.op('dve', fn, R=R, W=W)

        dve(lambda e: e.tensor_reduce(out=T['gmax'][:, :], in_=gl(), axis=AX.X, op=ALU.max), L_RES, ('gmax',))
        dve(lambda e: e.tensor_tensor(out=v3('goh', TT, 4), in0=gl(), in1=T['gmax'][:, :].unsqueeze(2).to_broadcast([128, TT, 4]),
                                      op=ALU.is_equal), L_RES + ('gmax',), ('goh',))
        dve(lambda e: e.tensor_tensor(out=v3('gsh', TT, 4), in0=gl(), in1=T['gmax'][:, :].unsqueeze(2).to_broadcast([128, TT, 4]),
                                      op=ALU.subtract), L_RES + ('gmax',), ('gsh',))
        P.op('act', lambda e: e.activation(out=T['gex'][:, :], in_=T['gsh'][:, :], func=AF.Exp), R=('gsh',), W=('gex',))
        dve(lambda e: e.tensor_reduce(out=T['gsum'][:, :], in_=v3('gex', TT, 4), axis=AX.X, op=ALU.add), ('gex',), ('gsum',))
        dve(lambda e: e.reciprocal(out=T['gw'][:, :], in_=T['gsum'][:, :]), ('gsum',), ('gw',))
        dve(lambda e: e.tensor_tensor(out=v4('elm', TT, 4, 8), in0=el(),
                                      in1=v3('goh', TT, 4).unsqueeze(3).to_broadcast([128, TT, 4, 8]), op=ALU.mult),
            L_RES + ('goh',), ('elm',))
        dve(lambda e: e.tensor_reduce(out=v3('els', TT, 8), in_=v4('elm', TT, 4, 8).rearrange("p t g e -> p t e g"),
                                      axis=AX.X, op=ALU.add), ('elm',), ('els',))
        dve(lambda e: e.tensor_reduce(out=T['m1'][:, :], in_=v3('els', TT, 8), axis=AX.X, op=ALU.max), ('els',), ('m1',))
        dve(lambda e: e.tensor_tensor(out=v3('oh1', TT, 8), in0=v3('els', TT, 8),
                                      in1=T['m1'][:, :].unsqueeze(2).to_broadcast([128, TT, 8]), op=ALU.is_equal),
            ('els', 'm1'), ('oh1',))
        dve(lambda e: e.scalar_tensor_tensor(out=T['els2'][:, :], in0=T['oh1'][:, :], scalar=-1.0e30, in1=T['els'][:, :],
                                             op0=ALU.mult, op1=ALU.add), ('oh1', 'els'), ('els2',))
        dve(lambda e: e.tensor_reduce(out=T['m2'][:, :], in_=v3('els2', TT, 8), axis=AX.X, op=ALU.max), ('els2',), ('m2',))
        dve(lambda e: e.tensor_tensor(out=v3('oh2', TT, 8), in0=v3('els2', TT, 8),
                                      in1=T['m2'][:, :].unsqueeze(2).to_broadcast([128, TT, 8]), op=ALU.is_equal),
            ('els2', 'm2'), ('oh2',))
        dve(lambda e: e.tensor_tensor(out=T['dd'][:, :], in0=T['m2'][:, :], in1=T['m1'][:, :], op=ALU.subtract), ('m1', 'm2'), ('dd',))
        P.op('act', lambda e: e.activation(out=T['ed'][:, :], in_=T['dd'][:, :], func=AF.Exp), R=('dd',), W=('ed',))
        dve(lambda e: e.tensor_scalar(out=T['ed1'][:, :], in0=T['ed'][:, :], scalar1=1.0, scalar2=None, op0=ALU.add), ('ed',), ('ed1',))
        dve(lambda e: e.reciprocal(out=T['w1'][:, :], in_=T['ed1'][:, :]), ('ed1',), ('w1',))
        dve(lambda e: e.tensor_tensor(out=T['W1'][:, :], in0=T['w1'][:, :], in1=T['gw'][:, :], op=ALU.mult), ('w1', 'gw'), ('W1',))
        dve(lambda e: e.tensor_tensor(out=T['W2'][:, :], in0=T['ed'][:, :], in1=T['W1'][:, :], op=ALU.mult), ('ed', 'W1'), ('W2',))
        for a_, oh in (('A1', 'oh1'), ('A2', 'oh2')):
            dve(lambda e, a_=a_, oh=oh: e.tensor_tensor(
                out=v4(a_, TT, 4, 8), in0=v3('goh', TT, 4).unsqueeze(3).to_broadcast([128, TT, 4, 8]),
                in1=v3(oh, TT, 8).unsqueeze(2).to_broadcast([128, TT, 4, 8]), op=ALU.mult), ('goh', oh), (a_,))
        dve(lambda e: e.tensor_tensor(out=T['Asum'][:, :], in0=T['A1'][:, :], in1=T['A2'][:, :], op=ALU.add), ('A1', 'A2'), ('Asum',))
        dve(lambda e: e.tensor_copy(out=T['Abf'][:, :], in_=T['Asum'][:, :]), ('Asum',), ('Abf',))
        pR = next_psf()
        pT = next_psf()
        P.op('pe', lambda e, pR=pR: e.matmul(T[pR][:, :], lhsT=T['ustrict'][:, :], rhs=T['Abf'][:, :], start=True, stop=True),
             R=('ustrict', 'Abf'), W=(pR,))
        P.op('pe', lambda e, pT=pT: e.matmul(T[pT][:, :], lhsT=T['ones'][:, :], rhs=T['Abf'][:, :], start=True, stop=True),
             R=('ones', 'Abf'), W=(pT,))
        dve(lambda e: e.memset(T['CT'][:, 0:32], 0.0), (), ('CT',))
        for t in range(1, TT):
            dve(lambda e, t=t, pT=pT: e.tensor_tensor(out=T['CT'][:, t * 32:(t + 1) * 32], in0=T['CT'][:, (t - 1) * 32:t * 32],
                                                      in1=T[pT][:, (t - 1) * 32:t * 32], op=ALU.add), (pT, 'CT'), ('CT',))
        dve(lambda e, pR=pR: e.tensor_tensor(out=T['rank'][:, :], in0=T[pR][:, :], in1=T['CT'][:, :], op=ALU.add), (pR, 'CT'), ('rank',))
        dve(lambda e: e.tensor_scalar(out=T['ovf'][:, :], in0=T['rank'][:, :], scalar1=float(CAP), scalar2=BIGIDX,
                                      op0=ALU.is_ge, op1=ALU.mult), ('rank',), ('ovf',))
        dve(lambda e: e.tensor_tensor(out=T['pos'][:, :], in0=T['rank'][:, :], in1=T['ebase'][:, :], op=ALU.add), ('rank', 'ebase'), ('pos',))
        dve(lambda e: e.tensor_tensor(out=T['pos'][:, :], in0=T['pos'][:, :], in1=T['ovf'][:, :], op=ALU.add), ('pos', 'ovf'), ('pos',))
        for a_, df, di in (('A1', 'd1f', 'd1i'), ('A2', 'd2f', 'd2i')):
            dve(lambda e, a_=a_: e.tensor_tensor(out=T['pm1'][:, :], in0=T[a_][:, :], in1=T['pos'][:, :], op=ALU.mult), (a_, 'pos'), ('pm1',))
            dve(lambda e, df=df: e.tensor_reduce(out=T[df][:, :], in_=v3('pm1', TT, 32), axis=AX.X, op=ALU.add), ('pm1',), (df,))
            dve(lambda e, df=df, di=di: e.tensor_copy(out=T[di][:, :], in_=T[df][:, :]), (df,), (di,))

        dve(lambda e, pT=pT: e.tensor_tensor(out=T['cnt'][:, :], in0=T['CT'][:, (TT - 1) * 32:TT * 32],
                                             in1=T[pT][:, (TT - 1) * 32:TT * 32], op=ALU.add), (pT, 'CT'), ('cnt',))
        dve(lambda e: e.tensor_tensor(out=T['bovf'][:, :].rearrange("p (e j) -> p e j", j=2),
                                      in0=T['posb'][:, :].rearrange("p (e j) -> p e j", j=2),
                                      in1=T['cnt'][:, :].unsqueeze(2).to_broadcast([128, NE, 2]), op=ALU.is_ge),
            ('posb', 'cnt'), ('bovf',))
        dve(lambda e: e.scalar_tensor_tensor(out=T['bovf'][:, :], in0=T['bovf'][:, :], scalar=BIGIDX, in1=T['slotb'][:, :],
                                             op0=ALU.mult, op1=ALU.add), ('bovf', 'slotb'), ('bovf',))
        dve(lambda e: e.tensor_copy(out=T['bidx'][:, :], in_=T['bovf'][:, :]), ('bovf',), ('bidx',))

        SC_RES = []

        for t in range(TT):
            s = t % 3
            s2 = t % 3
            if t + 2 < TT:
                hload(t + 2)
            early_step()
            P.op('act', lambda e, s=s, s2=s2: e.copy(out=T['hbb%d' % s2][:, :], in_=T['hl%d' % s][:, :]),
                 R=('hl%d' % s,), W=('hbb%d' % s2,))
            for k, di in ((1, 'd1i'), (2, 'd2i')):
                res = 'rows_s%d_%d' % (t, k)
                SC_RES.append(res)
                P.op('pool', lambda e, t=t, s2=s2, di=di: e.indirect_dma_start(
                    out=Dm['rows'][:, :], out_offset=bass.IndirectOffsetOnAxis(ap=T[di][:, t:t + 1], axis=0),
                    in_=T['hbb%d' % s2][:, :], in_offset=None, bounds_check=T['bcreg'], oob_is_err=False),
                    R=('hbb%d' % s2, di), W=(res,), dma='scat')
        SC_RES = tuple(SC_RES)
        while _casted[0] < len(_early):
            early_step()

        def wres(wb, sl):
            return ('%s%d_0' % (wb, sl), '%s%d_1' % (wb, sl))

        NB = NE * 2
        Y_RES = ['yrows%d' % b for b in range(NB)]

        def stA(b):
            r3 = b % 3
            slot0 = (b // 2) * CAP + (b % 2) * 128
            rw = 'RW%d' % r3
            pb = 'bk4'
            for kc in range(8):
                P.op('pe', lambda e, kc=kc: e.transpose(out=PB(pb)[:, kc * 128:(kc + 1) * 128],
                                                        in_=T[rw][:, kc * 128:(kc + 1) * 128],
                                                        identity=T['ident'][:, :]), R=(rw, 'ident'), W=(pb,))
            xt = 'XT%d' % r3
            P.op('dve', lambda e: e.tensor_copy(out=T[xt][:, :], in_=PB(pb)[:, :]), R=(pb,), W=(xt,))

        def rowload(b):
            r3 = b % 3
            slot0 = (b // 2) * CAP + (b % 2) * 128
            rw = 'RW%d' % r3
            P.op('pool', lambda e: e.indirect_dma_start(
                out=T[rw][:, :], out_offset=None, in_=Dm['rows'][:, :],
                in_offset=bass.IndirectOffsetOnAxis(ap=T['bidx'][:, b:b + 1], axis=0),
                bounds_check=T['bcreg'], oob_is_err=False), R=SC_RES + ('bidx',), W=(rw,), dma=rw)

        def stB1(b):
            r3 = b % 3
            sl = (b // 2) % 2
            xt = 'XT%d' % r3
            pg = rotB1()
            pu = rotB1()
            for (pp, wb) in ((pg, 'Wg'), (pu, 'Wu')):
                for kc in range(8):
                    P.op('pe', lambda e, kc=kc, pp=pp, wb=wb: e.matmul(
                        T[pp][:, :], lhsT=T[xt][:, kc * 128:(kc + 1) * 128],
                        rhs=T['%s%d' % (wb, sl)][:, kc * 512:(kc + 1) * 512], start=(kc == 0), stop=(kc == 7)),
                        R=(xt,) + wres(wb, sl), W=(pp,))
            sgf = 'sgf%d' % (b % 2)
            P.op('act', lambda e: e.activation(out=T[sgf][:, :], in_=T[pg][:, :], func=AF.Silu), R=(pg,), W=(sgf,))
            hh_ = 'Hh%d' % r3
            P.op('dve', lambda e: e.tensor_tensor(out=T[hh_][:, :], in0=T[pu][:, :], in1=T[sgf][:, :], op=ALU.mult),
                 R=(pu, sgf), W=(hh_,))

        def stB2(b):
            r3 = b % 3
            hh_ = 'Hh%d' % r3
            pb = 'bk5'
            for c in range(4):
                P.op('pe', lambda e, c=c: e.transpose(out=PB(pb)[:, c * 128:(c + 1) * 128],
                                                      in_=T[hh_][:, c * 128:(c + 1) * 128],
                                                      identity=T['ident'][:, :]), R=(hh_, 'ident'), W=(pb,))
            ht = 'HT%d' % r3
            P.op('act', lambda e: e.copy(out=T[ht][:, :], in_=PB(pb)[:, 0:512]), R=(pb,), W=(ht,))

        def stC(b):
            r3 = b % 3
            sl = (b // 2) % 2
            slot0 = (b // 2) * CAP + (b % 2) * 128
            ht = 'HT%d' % r3
            py = [rotC2(), rotC2()]
            for half in range(2):
                for c in range(4):
                    P.op('pe', lambda e, half=half, c=c: e.matmul(
                        T[py[half]][:, :], lhsT=T[ht][:, c * 128:(c + 1) * 128],
                        rhs=T['Wd%d' % sl][:, c * 1024 + half * 512: c * 1024 + (half + 1) * 512],
                        start=(c == 0), stop=(c == 3)), R=(ht,) + wres('Wd', sl), W=(py[half],))
            ys = 'YS%d' % r3
            P.op('act', lambda e: e.copy(out=T[ys][:, 0:512], in_=T[py[0]][:, :]), R=(py[0],), W=(ys + 'a',))
            P.op('dve', lambda e: e.tensor_copy(out=T[ys][:, 512:1024], in_=T[py[1]][:, :]), R=(py[1],), W=(ys + 'b',))
            P.op('pool', lambda e: e.indirect_dma_start(
                out=Dm['yrows'][:, :], out_offset=bass.IndirectOffsetOnAxis(ap=T['bidx'][:, b:b + 1], axis=0),
                in_=T[ys][:, :], in_offset=None, bounds_check=T['bcreg'], oob_is_err=False),
                R=(ys + 'a', ys + 'b', 'bidx'), W=(Y_RES[b], ys + 'a', ys + 'b'), dma='yst%d' % r3)

        def st0(i):
            if i >= EARLY + 1:
                for pc in pieces_at(i):
                    piece_cast(pc)
            if 0 <= i + 1 < NB:
                rowload(i + 1)
            if i + 1 >= EARLY + 1:
                for pc in pieces_at(i + 1):
                    piece_dma(pc)

        for i in range(-1, NB + 3):
            lists = [captured(st0, i)]
            if 0 <= i < NB:
                lists.append(captured(stA, i))
            if 0 <= i - 1 < NB:
                lists.append(captured(stB1, i - 1))
            if 0 <= i - 2 < NB:
                lists.append(captured(stB2, i - 2))
            if 0 <= i - 3 < NB:
                lists.append(captured(stC, i - 3))
            if MERGE_B:
                P.run_merged(lists)
            else:
                for l in lists:
                    for it in l:
                        P.op(*it)
        Y_RES = tuple(Y_RES)

        def cload(t):
            s = t % 3
            hl = 'hl%d' % s
            P.op('sp', lambda e: e.dma_start(out=T[hl][:, :], in_=Dm['h32'][t * 128:(t + 1) * 128, :]),
                 R=H32_RES, W=(hl,), dma=hl)
            for (yy, di) in (('Y1_%d' % s, 'd1i'), ('Y2_%d' % s, 'd2i')):
                if t < 3:
                    P.op('pool', lambda e, yy=yy: e.memset(T[yy][:, :], 0.0), R=(), W=(yy,))
                P.op('pool', lambda e, yy=yy, di=di: e.indirect_dma_start(
                    out=T[yy][:, :], out_offset=None, in_=Dm['yrows'][:, :],
                    in_offset=bass.IndirectOffsetOnAxis(ap=T[di][:, t:t + 1], axis=0),
                    bounds_check=T['bcreg'], oob_is_err=False), R=Y_RES + (di,), W=(yy,), dma='g' + yy)

        cload(0)
        cload(1)
        for t in range(TT):
            s = t % 3
            hl = 'hl%d' % s
            y1 = 'Y1_%d' % s
            y2 = 'Y2_%d' % s
            if t + 2 < TT:
                cload(t + 2)
            P.op('act', lambda e, hl=hl: e.mul(out=T[hl][:, :], in_=T[hl][:, :], mul=ALPHA), R=(hl,), W=(hl,))
            P.op('dve', lambda e, hl=hl, y1=y1, t=t: e.scalar_tensor_tensor(
                out=T[hl][:, :], in0=T[y1][:, :], scalar=T['W1'][:, t:t + 1], in1=T[hl][:, :], op0=ALU.mult, op1=ALU.add),
                R=(hl, y1, 'W1'), W=(hl,))
            P.op('dve', lambda e, hl=hl, y2=y2, t=t: e.scalar_tensor_tensor(
                out=T[hl][:, :], in0=T[y2][:, :], scalar=T['W2'][:, t:t + 1], in1=T[hl][:, :], op0=ALU.mult, op1=ALU.add),
                R=(hl, y2, 'W2'), W=(hl,))
            for half in range(2):
                P.op('dve', lambda e, hl=hl, half=half: e.bn_stats(out=T['lst2'][:, half * 6:(half + 1) * 6],
                                                                   in_=T[hl][:, half * 512:(half + 1) * 512]),
                     R=(hl,), W=('lst2_%d' % half,))
            P.op('dve', lambda e: e.bn_aggr(out=T['lmv2'][:, :], in_=T['lst2'][:, :]), R=('lst2_0', 'lst2_1'), W=('lmv2',))
            P.op('act', lambda e: e.activation(out=T['lsq2'][:, :], in_=T['lmv2'][:, 1:2], func=AF.Sqrt, bias=T['epsl'][:, 0:1]),
                 R=('lmv2', 'epsl'), W=('lsq2',))
            P.op('dve', lambda e, hl=hl: e.scalar_tensor_tensor(out=T[hl][:, :], in0=T[hl][:, :], scalar=T['lmv2'][:, 0:1],
                                                                in1=T['ln2g'][:, :], op0=ALU.subtract, op1=ALU.mult),
                 R=(hl, 'lmv2', 'ln2g'), W=(hl,))
            P.op('dve', lambda e: e.reciprocal(out=T['lrs2'][:, :], in_=T['lsq2'][:, :]), R=('lsq2',), W=('lrs2',))
            P.op('dve', lambda e, hl=hl: e.scalar_tensor_tensor(out=T[hl][:, :], in0=T[hl][:, :], scalar=T['lrs2'][:, 0:1],
                                                                in1=T['ln2b'][:, :], op0=ALU.mult, op1=ALU.add),
                 R=(hl, 'lrs2', 'ln2b'), W=(hl,))
            P.op('sp', lambda e, t=t, hl=hl: e.dma_start(out=Dm['out'][t * 128:(t + 1) * 128, :], in_=T[hl][:, :]),
                 R=(hl,), W=('out%d' % t,), dma='outst')


    P.barrier(engines=['sp'])
    P.analyze()

    from contextlib import ExitStack
    sem_names = ['eng_' + e for e in ENGINES] + ['dma_' + k for k in P.dma_cnt.keys()]
    with ExitStack() as cstack:
        sems = {sn: cstack.enter_context(nc.semaphore(sn)) for sn in sem_names}
        for i in range(8):
            T['bk%d' % i] = cstack.enter_context(nc.psum_tensor('bk%d' % i, [128, 512], F32))
        for name, (shape, dt, scope) in specs.items():
            if scope == 'c':
                T[name] = cstack.enter_context(nc.sbuf_tensor('s_' + name, shape, dt))

        def emit_phase(phase, scope):
            with ExitStack() as pstack:
                for name, (shape, dt, sc) in specs.items():
                    if sc == scope:
                        T[name] = pstack.enter_context(nc.sbuf_tensor('s_' + name, shape, dt))
                with nc.Block() as block:
                    def runner(engname):
                        def f(e):
                            for o in P.eng_ops[engname]:
                                if o.phase != phase:
                                    continue
                                for (sname, val) in o.waits:
                                    e.wait_ge(sems[sname], val)
                                if o.fn is not None:
                                    ins = o.fn(e)
                                    if o.is_dma:
                                        ins.then_inc(sems['dma_' + o.key], 16)
                                    elif o.signal:
                                        ins.then_inc(sems['eng_' + engname], 1)
                        return f
                    block.tensor(runner('pe'))
                    block.scalar(runner('act'))
                    block.vector(runner('dve'))
                    block.gpsimd(runner('pool'))
                    block.sync(runner('sp'))

        emit_phase(0, 'a')
        if stage >= 2:
            emit_phase(1, 'b')
    return nc


_NC_CACHE = {}


def _prep_shared(w_in, ret_gn_g, attn_sinks, w_out, ln1_g, ln1_b, w_group_router, b_group_router,
                 w_expert_router, b_expert_router, w_gate, w_up, w_down, ln2_g, ln2_b):
    f = np.float32
    sh = dict(_consts())
    w_in = np.asarray(w_in[0], f)
    cols = np.arange(INW)
    qa = np.zeros(512, np.int64)
    for j in range(4):
        for kh in range(2):
            qa[j * 128 + kh * 64: j * 128 + (kh + 1) * 64] = 2048 + kh * 256 + j * 64 + np.arange(64)
    cols[2048:2560] = qa
    sh['w_in'] = np.ascontiguousarray(w_in[:, cols])
    sh['w_out'] = np.ascontiguousarray(np.asarray(w_out[0], f))
    sh['w_gate'] = np.ascontiguousarray(np.asarray(w_gate[0], f))
    sh['w_up'] = np.ascontiguousarray(np.asarray(w_up[0], f))
    sh['w_down'] = np.ascontiguousarray(np.asarray(w_down[0], f))
    bc = lambda v: np.ascontiguousarray(np.broadcast_to(np.asarray(v, f).reshape(1, -1), (128, np.asarray(v).size)))
    sh['gng'] = bc(ret_gn_g[0])
    sk = np.zeros((128, 4), f)
    sk[0:64, :] = np.asarray(attn_sinks[0], f)[0:4][None, :]
    sk[64:128, :] = np.asarray(attn_sinks[0], f)[4:8][None, :]
    sh['sk'] = sk
    sh['sk8'] = bc(attn_sinks[0])
    sh['ln1g'] = bc(ln1_g[0])
    sh['ln1b'] = bc(ln1_b[0])
    sh['ln2g'] = bc(ln2_g[0])
    sh['ln2b'] = bc(ln2_b[0])
    wr = np.concatenate([np.asarray(w_group_router[0], f)] +
                        [np.asarray(w_expert_router[0][g], f) for g in range(4)], axis=1)
    sh['wr'] = np.ascontiguousarray(wr)
    brv = np.concatenate([np.asarray(b_group_router[0], f).reshape(-1), np.asarray(b_expert_router[0], f).reshape(-1)])
    sh['br'] = bc(brv)
    return sh


def kernel(x, w_in, ret_gn_g, attn_sinks, w_out, ln1_g, ln1_b, w_group_router, b_group_router,
           w_expert_router, b_expert_router, w_gate, w_up, w_down, ln2_g, ln2_b, _stage=2):
    if _stage not in _NC_CACHE:
        _NC_CACHE[_stage] = build(_stage)
    nc = _NC_CACHE[_stage]
    sh = _prep_shared(w_in, ret_gn_g, attn_sinks, w_out, ln1_g, ln1_b, w_group_router, b_group_router,
                      w_expert_router, b_expert_router, w_gate, w_up, w_down, ln2_g, ln2_b)
    x = np.asarray(x, np.float32)
    in_maps = []
    for c in range(8):
        m = dict(sh)
        m['x'] = np.ascontiguousarray(x[c])
        in_maps.append(m)
    res = run_bass_kernel_spmd(nc, in_maps, core_ids=list(range(8)))
    key = 'h32' if _stage == 1 else 'out'
    return np.stack([np.asarray(r[key], np.float32) for r in res.results], axis=0)
```
